# Optimizing a Trainium2 kernel written in Bass

```python
import jax, jax.numpy as jnp
from jax import lax
import numpy as np

D_MODEL = 1024
BATCH = 8
SEQ = 2048
DEPTH = 2
DEC_BATCH = 32
DEC_SEQ = 1
PAST_LEN = 8192
PAGE_SIZE = 128

N_HEADS = 8
HEAD_DIM = 64
ATTN_WIDTH = N_HEADS * HEAD_DIM
D_RNN = D_MODEL
N_LRU_BLOCKS = 8
LRU_BLOCK = D_RNN // N_LRU_BLOCKS
CONV_WIDTH = 4
LRU_C = 8.0
D_FF = 7 * D_MODEL // 2
N_EXPERTS = 8
TOP_K = 2
D_PLE = 256
Q_BLOCK = 128
EPS = 1e-6
NEG_INF = -1e30
SPLIT_SIZES = (ATTN_WIDTH, ATTN_WIDTH, ATTN_WIDTH, N_HEADS, D_RNN, D_RNN, D_MODEL, D_MODEL)
N_IN = sum(SPLIT_SIZES)
N_DENSE = (DEPTH + 1) // 2
N_MOE = DEPTH // 2

kernel_name = "fox_rglru_gated_hybrid_step"


def rmsnorm(x, g):
    xf = x.astype(jnp.float32)
    y = xf * lax.rsqrt(jnp.mean(xf * xf, axis=-1, keepdims=True) + EPS) * g.astype(jnp.float32)
    return y.astype(x.dtype)


def fox_attend(q, k, v, cq, ck, q_pos, k_pos):
    s = jnp.einsum('bqhd,bkhd->bhqk', q, k).astype(jnp.float32) * (HEAD_DIM ** -0.5)
    s = s + jnp.transpose(cq, (0, 2, 1))[:, :, :, None] - jnp.transpose(ck, (0, 2, 1))[:, :, None, :]
    mask = k_pos[None, :] <= q_pos[:, None]
    s = jnp.where(mask[None, None], s, NEG_INF)
    p = jax.nn.softmax(s, axis=-1).astype(v.dtype)
    return jnp.einsum('bhqk,bkhd->bqhd', p, v)


def attn_prompt(q, k, v, logf):
    B, T, H, Dh = q.shape
    nb = T // Q_BLOCK
    c = jnp.cumsum(logf, axis=1)
    pos = jnp.arange(T)
    qb = q.reshape(B, nb, Q_BLOCK, H, Dh).swapaxes(0, 1)
    cb = c.reshape(B, nb, Q_BLOCK, H).swapaxes(0, 1)
    pb = pos.reshape(nb, Q_BLOCK)

    def one(blk):
        qi, ci, pi = blk
        return fox_attend(qi, k, v, ci, c, pi, pos)

    o = lax.map(one, (qb, cb, pb))
    return o.swapaxes(0, 1).reshape(B, T, H * Dh)


def attn_with_past(q, k, v, logf, past_k, past_v, past_logf):
    B, T, H, Dh = q.shape
    k_all = jnp.concatenate([past_k, k], axis=1)
    v_all = jnp.concatenate([past_v, v], axis=1)
    c = jnp.cumsum(jnp.concatenate([past_logf.astype(jnp.float32), logf], axis=1), axis=1)
    L = c.shape[1]
    past = L - T
    q_pos = past + jnp.arange(T)
    k_pos = jnp.arange(L)
    o = fox_attend(q, k_all, v_all, c[:, past:], c, q_pos, k_pos)
    return o.reshape(B, T, H * Dh)


def lru_combine(e1, e2):
    a1, b1 = e1
    a2, b2 = e2
    return a1 * a2, a2 * b1 + b2


def rg_lru_branch(xr, gr, conv_prev, h0, conv_w, conv_b, w_rg, b_rg, w_ig, b_ig, lam):
    B, T, C = xr.shape
    xp = jnp.concatenate([conv_prev.astype(xr.dtype), xr], axis=1)
    xc = conv_b + sum(xp[:, j:j + T] * conv_w[j] for j in range(CONV_WIDTH))
    new_conv = xp[:, T:]
    xb = xc.reshape(B, T, N_LRU_BLOCKS, LRU_BLOCK)
    r = jax.nn.sigmoid((jnp.einsum('btnc,ncd->btnd', xb, w_rg).reshape(B, T, C) + b_rg).astype(jnp.float32))
    i = jax.nn.sigmoid((jnp.einsum('btnc,ncd->btnd', xb, w_ig).reshape(B, T, C) + b_ig).astype(jnp.float32))
    log_a = -LRU_C * r * jax.nn.softplus(-lam.astype(jnp.float32))
    a = jnp.exp(log_a)
    bx = jnp.sqrt(-jnp.expm1(2.0 * log_a)) * (i * xc.astype(jnp.float32))
    A, Bc = lax.associative_scan(lru_combine, (a, bx), axis=1)
    h = A * h0.astype(jnp.float32)[:, None] + Bc
    y = (h * jax.nn.gelu(gr.astype(jnp.float32))).astype(xr.dtype)
    return y, new_conv, h[:, -1]


def swiglu(x, wg, wu, wd):
    return (jax.nn.silu(x @ wg) * (x @ wu)) @ wd


def moe_ffn(xn, router, wg, wu, wd):
    B, T, D = xn.shape
    xt = xn.reshape(B * T, D)
    logits = (xt @ router).astype(jnp.float32)
    top_v, top_i = lax.top_k(logits, TOP_K)
    w = jax.nn.softmax(top_v, axis=-1)
    gates = jnp.sum(jax.nn.one_hot(top_i, N_EXPERTS, dtype=jnp.float32) * w[..., None], axis=1)
    out = jnp.zeros_like(xt)
    for e in range(N_EXPERTS):
        out = out + gates[:, e:e + 1].astype(xt.dtype) * swiglu(xt, wg[e], wu[e], wd[e])
    return out.reshape(B, T, D)


def trunk_layer(x, p_l, l, lw, past, conv_prev, h0):
    B, T, _ = x.shape
    xn = rmsnorm(x, lw['g_mix'])
    proj = xn @ lw['w_in']
    idx = list(np.cumsum(SPLIT_SIZES)[:-1])
    q, k, v, fl, xr, gr, ga, gb = jnp.split(proj, idx, axis=-1)
    q = q.reshape(B, T, N_HEADS, HEAD_DIM)
    k = k.reshape(B, T, N_HEADS, HEAD_DIM)
    v = v.reshape(B, T, N_HEADS, HEAD_DIM)
    logf = jax.nn.log_sigmoid(fl.astype(jnp.float32) + lw['b_f'].astype(jnp.float32))
    if past is None:
        attn = attn_prompt(q, k, v, logf)
    else:
        attn = attn_with_past(q, k, v, logf, past[0], past[1], past[2])
    y_rnn, conv_new, h_new = rg_lru_branch(xr, gr, conv_prev, h0, lw['conv_w'], lw['conv_b'],
                                           lw['w_rg'], lw['b_rg'], lw['w_ig'], lw['b_ig'], lw['lam'])
    merged = jax.nn.sigmoid(ga) * (attn @ lw['w_a_up']) + jax.nn.sigmoid(gb) * (y_rnn @ lw['w_b_up'])
    x = x + merged @ lw['w_out']
    xn = rmsnorm(x, lw['g_ffn'])
    if l % 2 == 0:
        x = x + swiglu(xn, lw['wg'], lw['wu'], lw['wd'])
    else:
        x = x + moe_ffn(xn, lw['router'], lw['wg'], lw['wu'], lw['wd'])
    gate = jax.nn.sigmoid(rmsnorm(x, lw['g_ple']) @ lw['w_ple_gate'])
    x = x + gate * (p_l @ lw['w_ple_proj'])
    return x, k, v, logf, conv_new, h_new


def setup_inputs(seed: int = 0) -> dict:
    key = jax.random.key(seed)
    ks = iter(jax.random.split(key, 64))
    f32 = jnp.float32
    nrm = lambda shape, s: jax.random.normal(next(ks), shape, f32) * s
    n_pages = PAST_LEN // PAGE_SIZE
    n_used = DEC_BATCH * n_pages
    n_pool = n_used + max(1, n_used // 4)
    u = jax.random.uniform(next(ks), (DEPTH, D_RNN), f32, 0.9, 0.999)
    s_lam = u ** (1.0 / LRU_C)
    lam = jnp.log(s_lam) - jnp.log1p(-s_lam)
    page_table = jax.random.permutation(next(ks), n_pool)[:n_used].reshape(DEC_BATCH, n_pages).astype(jnp.int32)
    return {
        'x_prompt': nrm((BATCH, SEQ, D_MODEL), 1.0),
        'x_sample': nrm((DEC_BATCH, DEC_SEQ, D_MODEL), 1.0),
        'p_prompt': nrm((DEPTH, BATCH, SEQ, D_PLE), 1.0),
        'p_sample': nrm((DEPTH, DEC_BATCH, DEC_SEQ, D_PLE), 1.0),
        'cache_k': nrm((DEPTH, n_pool, PAGE_SIZE, N_HEADS, HEAD_DIM), 1.0),
        'cache_v': nrm((DEPTH, n_pool, PAGE_SIZE, N_HEADS, HEAD_DIM), 1.0),
        'cache_logf': jax.nn.log_sigmoid(nrm((DEPTH, n_pool, PAGE_SIZE, N_HEADS), 1.0) + 2.0),
        'state_conv': nrm((DEPTH, DEC_BATCH, CONV_WIDTH - 1, D_RNN), 1.0),
        'state_h': nrm((DEPTH, DEC_BATCH, D_RNN), 0.5),
        'page_table': page_table,
        'g_mix': 1.0 + nrm((DEPTH, D_MODEL), 0.02),
        'w_in': nrm((DEPTH, D_MODEL, N_IN), D_MODEL ** -0.5),
        'b_f': 2.0 + nrm((DEPTH, N_HEADS), 0.1),
        'conv_w': nrm((DEPTH, CONV_WIDTH, D_RNN), CONV_WIDTH ** -0.5),
        'conv_b': nrm((DEPTH, D_RNN), 0.01),
        'w_rg': nrm((DEPTH, N_LRU_BLOCKS, LRU_BLOCK, LRU_BLOCK), LRU_BLOCK ** -0.5),
        'b_rg': nrm((DEPTH, D_RNN), 0.01),
        'w_ig': nrm((DEPTH, N_LRU_BLOCKS, LRU_BLOCK, LRU_BLOCK), LRU_BLOCK ** -0.5),
        'b_ig': nrm((DEPTH, D_RNN), 0.01),
        'lru_lambda': lam,
        'w_a_up': nrm((DEPTH, ATTN_WIDTH, D_MODEL), ATTN_WIDTH ** -0.5),
        'w_b_up': nrm((DEPTH, D_RNN, D_MODEL), D_RNN ** -0.5),
        'w_out': nrm((DEPTH, D_MODEL, D_MODEL), D_MODEL ** -0.5),
        'g_ffn': 1.0 + nrm((DEPTH, D_MODEL), 0.02),
        'dense_wg': nrm((N_DENSE, D_MODEL, D_FF), D_MODEL ** -0.5),
        'dense_wu': nrm((N_DENSE, D_MODEL, D_FF), D_MODEL ** -0.5),
        'dense_wd': nrm((N_DENSE, D_FF, D_MODEL), D_FF ** -0.5),
        'moe_router': nrm((N_MOE, D_MODEL, N_EXPERTS), D_MODEL ** -0.5),
        'moe_wg': nrm((N_MOE, N_EXPERTS, D_MODEL, D_FF), D_MODEL ** -0.5),
        'moe_wu': nrm((N_MOE, N_EXPERTS, D_MODEL, D_FF), D_MODEL ** -0.5),
        'moe_wd': nrm((N_MOE, N_EXPERTS, D_FF, D_MODEL), D_FF ** -0.5),
        'g_ple': 1.0 + nrm((DEPTH, D_MODEL), 0.02),
        'w_ple_gate': nrm((DEPTH, D_MODEL, D_MODEL), D_MODEL ** -0.5),
        'w_ple_proj': nrm((DEPTH, D_PLE, D_MODEL), D_PLE ** -0.5),
        'g_final': 1.0 + nrm((D_MODEL,), 0.02),
    }


def reference(x_prompt, x_sample, p_prompt, p_sample, cache_k, cache_v, cache_logf, state_conv, state_h,
              page_table, g_mix, w_in, b_f, conv_w, conv_b, w_rg, b_rg, w_ig, b_ig, lru_lambda,
              w_a_up, w_b_up, w_out, g_ffn, dense_wg, dense_wu, dense_wd, moe_router, moe_wg, moe_wu,
              moe_wd, g_ple, w_ple_gate, w_ple_proj, g_final):
    xp, xs = x_prompt, x_sample
    DB = x_sample.shape[0]
    kp, vp, lp, cp, hp = [], [], [], [], []
    ksm, vsm, lsm, csm, hsm = [], [], [], [], []
    for l in range(DEPTH):
        lw = {'g_mix': g_mix[l], 'w_in': w_in[l], 'b_f': b_f[l], 'conv_w': conv_w[l], 'conv_b': conv_b[l],
              'w_rg': w_rg[l], 'b_rg': b_rg[l], 'w_ig': w_ig[l], 'b_ig': b_ig[l], 'lam': lru_lambda[l],
              'w_a_up': w_a_up[l], 'w_b_up': w_b_up[l], 'w_out': w_out[l], 'g_ffn': g_ffn[l],
              'g_ple': g_ple[l], 'w_ple_gate': w_ple_gate[l], 'w_ple_proj': w_ple_proj[l]}
        m = l // 2
        if l % 2 == 0:
            lw['wg'], lw['wu'], lw['wd'] = dense_wg[m], dense_wu[m], dense_wd[m]
        else:
            lw['router'] = moe_router[m]
            lw['wg'], lw['wu'], lw['wd'] = moe_wg[m], moe_wu[m], moe_wd[m]
        conv0 = jnp.zeros((xp.shape[0], CONV_WIDTH - 1, D_RNN), xp.dtype)
        h0 = jnp.zeros((xp.shape[0], D_RNN), jnp.float32)
        xp, k1, v1, f1, c1, h1 = trunk_layer(xp, p_prompt[l], l, lw, None, conv0, h0)
        kp.append(k1); vp.append(v1); lp.append(f1); cp.append(c1); hp.append(h1)
        past_k = cache_k[l, page_table].reshape(DB, -1, N_HEADS, HEAD_DIM)
        past_v = cache_v[l, page_table].reshape(DB, -1, N_HEADS, HEAD_DIM)
        past_f = cache_logf[l, page_table].reshape(DB, -1, N_HEADS)
        xs, k2, v2, f2, c2, h2 = trunk_layer(xs, p_sample[l], l, lw, (past_k, past_v, past_f),
                                             state_conv[l], state_h[l])
        ksm.append(k2); vsm.append(v2); lsm.append(f2); csm.append(c2); hsm.append(h2)
    y_prompt = rmsnorm(xp, g_final)
    y_sample = rmsnorm(xs, g_final)
    return (y_prompt, y_sample,
            jnp.stack(kp), jnp.stack(vp), jnp.stack(lp), jnp.stack(cp), jnp.stack(hp),
            jnp.stack(ksm), jnp.stack(vsm), jnp.stack(lsm), jnp.stack(csm), jnp.stack(hsm))
```

```python
import contextlib
import types
import numpy as np
import concourse.bass as bass
import concourse.mybir as mybir
from concourse.bass_utils import run_bass_kernel_spmd

F32 = mybir.dt.float32
BF16 = mybir.dt.bfloat16
I32 = mybir.dt.int32
AF = mybir.ActivationFunctionType
ALU = mybir.AluOpType
AX = mybir.AxisListType

NCORES = 8
D = 1024
KC = 8
T = 2048
NS = 4
NTOK = T + NS
TILES = [(0, 512), (512, 512), (1024, 512), (1536, 512), (2048, NS)]
NH = 8
DH = 64
DFF = 3584
FCH = DFF // 128
NEXP = 8
DPLE = 256
NIN = 5640
EPS = 1e-6
NPG = 64
NPOOL = 2560
DEPTH = 2
OQ, OK_, OV, OFL, OXR, OGR, OGA, OGB = 0, 512, 1024, 1536, 1544, 2568, 3592, 4616
V_GMIX, V_CW, V_CB, V_BRG, V_BIG, V_LAM, V_GFFN, V_GPLE, LV = 0, 8, 40, 48, 56, 64, 72, 80, 88
V_GFIN = DEPTH * LV
NV = V_GFIN + 8


def _freeze(fn):
    if fn.__closure__ is None:
        return fn
    cells = []
    for c in fn.__closure__:
        try:
            cells.append(types.CellType(c.cell_contents))
        except ValueError:
            cells.append(c)
    return types.FunctionType(fn.__code__, fn.__globals__, fn.__name__, fn.__defaults__, tuple(cells))


class Op:
    __slots__ = ("eng", "fn", "deps", "signaled", "seq", "is_dma", "sem", "val", "idx")

    def __init__(self, eng, fn, deps, is_dma):
        self.eng, self.fn, self.deps, self.is_dma = eng, fn, deps, is_dma
        self.signaled, self.seq, self.sem, self.val, self.idx = False, 0, None, 0, 0


class Sched:
    ENGS = ("pe", "act", "dve", "pool", "sp")
    NDMASEM = 12

    def __init__(self, nc):
        self.nc = nc
        self.ops = {e: [] for e in self.ENGS}
        self.last_w, self.readers = {}, {}
        self.dma_count = {e: 0 for e in self.ENGS}
        self.dma_ops = {e: [] for e in self.ENGS}

    def _hazards(self, reads, writes):
        deps = []
        for k in reads:
            w = self.last_w.get(k)
            if w is not None:
                deps.append(w)
        for k in writes:
            w = self.last_w.get(k)
            if w is not None:
                deps.append(w)
            deps.extend(self.readers.get(k, ()))
        return deps

    def _commit(self, op, reads, writes):
        for k in reads:
            self.readers.setdefault(k, []).append(op)
        for k in writes:
            self.last_w[k] = op
            self.readers[k] = []

    def op(self, eng, fn, reads=(), writes=(), deps=()):
        o = Op(eng, _freeze(fn), list(deps) + self._hazards(reads, writes), False)
        self.ops[eng].append(o)
        self._commit(o, reads, writes)
        return o

    def dma(self, eng, out, in_, reads=(), writes=(), deps=(), **kw):
        d = list(deps) + self._hazards(reads, writes)
        i = self.dma_count[eng]
        self.dma_count[eng] += 1
        if i >= self.NDMASEM:
            d.append(self.dma_ops[eng][i - self.NDMASEM])
        o = Op(eng, lambda e: e.dma_start(out=out, in_=in_, **kw), d, True)
        o.idx = i
        self.dma_ops[eng].append(o)
        self.ops[eng].append(o)
        self._commit(o, reads, writes)
        return o

    def dma_fn(self, eng, fn, reads=(), writes=(), deps=()):
        d = list(deps) + self._hazards(reads, writes)
        i = self.dma_count[eng]
        self.dma_count[eng] += 1
        if i >= self.NDMASEM:
            d.append(self.dma_ops[eng][i - self.NDMASEM])
        o = Op(eng, _freeze(fn), d, True)
        o.idx = i
        self.dma_ops[eng].append(o)
        self.ops[eng].append(o)
        self._commit(o, reads, writes)
        return o

    def emit(self, final_ops):
        nc = self.nc
        for e in self.ENGS:
            for o in self.ops[e]:
                for d in o.deps:
                    if d.is_dma or (d.eng == "pe" and o.eng == "pe" and not o.is_dma):
                        continue
                    d.signaled = True
        for o in final_ops:
            if not o.is_dma:
                o.signaled = True
        for e in self.ENGS:
            n = 0
            for o in self.ops[e]:
                if not o.is_dma and o.signaled:
                    n += 1
                    o.seq = n
        with contextlib.ExitStack() as st:
            esem = {e: st.enter_context(nc.semaphore("s_" + e)) for e in self.ENGS}
            dsem = {e: [st.enter_context(nc.semaphore("d_%s%d" % (e, i))) for i in range(self.NDMASEM)]
                    for e in self.ENGS if self.dma_count[e] > 0}
            for e in self.ENGS:
                for o in self.ops[e]:
                    if o.is_dma:
                        o.sem, o.val = dsem[e][o.idx % self.NDMASEM], 16 * (o.idx // self.NDMASEM + 1)
                    else:
                        o.sem, o.val = esem[e], o.seq
            block = st.enter_context(nc.Block())

            def run(engname, engobj):
                waited = {}
                for o in self.ops[engname]:
                    need = {}
                    for d in o.deps:
                        if (not d.is_dma) and d.eng == "pe" and engname == "pe" and not o.is_dma:
                            continue
                        key = id(d.sem)
                        if waited.get(key, 0) >= d.val:
                            continue
                        if key not in need or need[key][1] < d.val:
                            need[key] = (d.sem, d.val)
                    for key, (s, v) in need.items():
                        engobj.wait_ge(s, v)
                        waited[key] = v
                    inst = o.fn(engobj)
                    if o.is_dma:
                        inst.then_inc(o.sem, 16)
                    elif o.signaled:
                        inst.then_inc(o.sem, 1)
                if engname == "sp":
                    for o in final_ops:
                        key = id(o.sem)
                        if waited.get(key, 0) < o.val:
                            engobj.wait_ge(o.sem, o.val)
                            waited[key] = o.val

            block.tensor(lambda eng: run("pe", eng))
            block.scalar(lambda eng: run("act", eng))
            block.vector(lambda eng: run("dve", eng))
            block.gpsimd(lambda eng: run("pool", eng))
            block.sync(lambda eng: run("sp", eng))


def build_program(depth=DEPTH, stop_after=None):
    nc = bass.Bass("TRN2", target_bir_lowering=False)
    S = Sched(nc)
    global LAST_SCHED
    LAST_SCHED = S

    def din(name, shape, dt=F32):
        return nc.dram_tensor(name, list(shape), dt, kind="ExternalInput").ap()

    def dout(name, shape, dt=F32):
        return nc.dram_tensor(name, list(shape), dt, kind="ExternalOutput").ap()

    xT_d = din("xT", [D, NTOK])
    pT_d = din("pT", [DEPTH, DPLE, NTOK])
    scT_d = din("scT", [DEPTH, D, 3, NS])
    shT_d = din("shT", [DEPTH, D, NS])
    vecs_d = din("vecs", [128, NV])
    bfT_d = din("bfT", [NH, DEPTH])
    w_in_d = din("w_in", [DEPTH, D, NIN])
    w_rg_d = din("w_rg", [DEPTH, KC, 128, 128])
    w_ig_d = din("w_ig", [DEPTH, KC, 128, 128])
    w_a_d = din("w_a_up", [DEPTH, 512, D])
    w_b_d = din("w_b_up", [DEPTH, D, D])
    w_o_d = din("w_out", [DEPTH, D, D])
    dwg_d = din("dense_wg", [D, DFF])
    dwu_d = din("dense_wu", [D, DFF])
    dwd_d = din("dense_wd", [DFF, D])
    rt_d = din("router", [D, NEXP])
    mwg_d = din("moe_wg", [NEXP, D, DFF])
    mwu_d = din("moe_wu", [NEXP, D, DFF])
    mwd_d = din("moe_wd", [NEXP, DFF, D])
    wpg_d = din("w_ple_gate", [DEPTH, D, D])
    wpp_d = din("w_ple_proj", [DEPTH, DPLE, D])
    cst_d = din("cst", [128, 5 * 128])
    e8_d = din("e8", [8, 8 * 128])
    oh_d = din("oh", [NH, 8, T])
    ck_d = din("ck", [DEPTH, NPOOL, 128, 512])
    cv_d = din("cv", [DEPTH, NPOOL, 128, 512])
    clf_d = din("clf", [DEPTH, NPOOL, 128, NH])
    pt_d = din("pt", [1, NS * NPG], I32)
    cf_d = din("cf", [128, 256 + 2 * NH + 1])

    yT_o = dout("yT", [D, NTOK])
    kT_o = dout("kT", [DEPTH, 512, NTOK])
    v_o = dout("v", [DEPTH, NTOK, 512])
    lfT_o = dout("lfT", [DEPTH, NH, NTOK])
    cvT_o = dout("cvT", [DEPTH, D, 3])
    cvS_o = dout("cvS", [DEPTH, D, 3, NS])
    hT_o = dout("hT", [DEPTH, D, 1 + NS])
    finals = []

    with contextlib.ExitStack() as st:
        def sb(name, shape, dt):
            return st.enter_context(nc.sbuf_tensor(name, list(shape), dt))

        xT = sb("xT_s", [128, KC, NTOK], F32)
        xnT = sb("xnT", [128, KC, NTOK], BF16)
        R1 = sb("R1", [128, 4 * NTOK], BF16)
        R2 = sb("R2", [128, 18432], BF16)
        ring = [sb("ring%d" % i, [128, 4096], BF16) for i in range(3)]
        vecs = sb("vecs_s", [128, NV], F32)
        lsc = sb("lsc", [128, DEPTH * KC], F32)
        cst = sb("cst_s", [128, 5 * 128], BF16)
        identf = sb("identf", [128, 128], F32)
        cf = sb("cf_s", [128, 256 + 2 * NH + 1], F32)
        idx = sb("idx", [128, NS * NPG], I32)
        bfn = sb("bfn", [NH, DEPTH], F32)
        rstd = sb("rstd", [128, NTOK], F32)
        sqt = sb("sqt", [128, 1, 512], BF16)
        stg = [sb("stg%d" % i, [128, 512], F32) for i in range(2)]
        wgate = sb("wgate", [128, 2, KC, 128], BF16)
        carry = sb("carry", [128, KC, 3], F32)
        hcar = sb("hcar", [128, KC], F32)
        prevS = sb("prevS", [128, KC, 3, NS], F32)
        h0S = sb("h0S", [128, KC, NS], F32)
        xrS = sb("xrS", [128, KC, NS], F32)
        hS = sb("hS_s", [128, KC, 1 + NS], F32)
        c8b = sb("c8b", [NH, T], BF16)
        negc = sb("negc", [128, 16, NH], F32)
        rtw = sb("rtw", [128, KC, NEXP], F32)
        fsc = sb("fsc", [128, 4], F32)
        psb = [st.enter_context(nc.psum_tensor("ps%d" % i, [128, 512], F32)) for i in range(8)]

        ident = cst[:, 0:128]
        onesb = cst[:, 128:256]
        mneg = cst[:, 256:384]
        opad = [cst[:, 384:512], cst[:, 512:640]]
        utri = cf[:, 0:128]
        onesf = cf[:, 128:256]
        bfrow = cf[:, 256:256 + 2 * NH]

        def r2f(off, n):
            return R2[:, off:off + 2 * n].bitcast(F32)

        R2KEYS = (["y%d" % k for k in range(KC)] + ["t1", "t2", "t3", "xcb", "sgt", "qT", "kTb", "qTs", "kTs", "Vp", "attnS"]
                  + ["attn%d" % k for k in range(4)] + ["PT%d" % k for k in range(3)]
                  + ["sga", "tmpm", "sgf", "tf", "lgS", "gatesT", "Ge", "e8f", "gwk", "lgt", "m12", "sgp", "tp", "pTb",
                     "lfT", "cT", "ones8"] + ["xnf%d" % t for t in range(5)])
        RSKEYS = ["rstd%d" % t for t in range(5)]
        DKEYS = ["ptb", "ptf", "pg0", "pg1", "pg2", "pg3", "pg4", "pg5", "dX", "dSP", "dlat", "dqs", "dvwn", "dmisc"]
        R1KEYS = DKEYS + ["y%d" % k for k in range(KC)] + ["mg%d" % m for m in range(KC)] + ["h%d_%d" % (f_, t) for f_ in range(4) for t in range(5)]

        def fence(keys):
            S.op("dve", lambda e: e.memset(fsc[:], 0.0), writes=list(dict.fromkeys(keys)) + ["fsc"])

        psi = [0]
        psrot = [list(range(8))]

        def ps_gen():
            rot = psrot[0]
            i = rot[psi[0] % len(rot)]
            psi[0] += 1
            return psb[i], "ps%d" % i

        ringi = [0]

        def slab(parts):
            i = ringi[0] % len(ring)
            ringi[0] += 1
            key = "ring%d" % i
            for (off, k, n, src) in parts:
                dst = ring[i][:, off:off + k * n].rearrange("p (k n) -> p k n", k=k)
                S.dma("pool", dst, src, writes=[key])
            return ring[i], key

        def wview(w2d, c0, n):
            return w2d.rearrange("(k p) n -> p k n", p=128)[:, :, c0:c0 + n]

        def w3(buf, k, n=512, off=0):
            return buf[:, off:off + k * n].rearrange("p (k n) -> p k n", k=k)

        def xk(t, k):
            return "x%d_%d" % (k, t)

        def xnk(t, k):
            return "xn%d_%d" % (k, t)

        def xnkeys(t):
            return [xnk(t, k) for k in range(KC)]

        def proj(out_ps, pskey, lhs_fn, nk, rhs_fn, rkeys, wkey):
            for k in range(nk):
                L_, R_ = lhs_fn(k), rhs_fn(k)
                S.op("pe", (lambda e, k=k, L_=L_, R_=R_: e.matmul(out_ps, lhsT=L_, rhs=R_, start=(k == 0), stop=(k == nk - 1))),
                     reads=[wkey, rkeys[k]], writes=[pskey])

        S.dma("sp", xT[:], xT_d.rearrange("(k p) n -> p k n", p=128), writes=[xk(t, k) for t in range(5) for k in range(KC)])
        S.dma("sp", vecs[:], vecs_d, writes=["vecs"])
        S.dma("pool", cst[:], cst_d, writes=["cst"])
        S.dma("sp", identf[:], cst_d[:, 0:128], writes=["identf"])
        S.dma("sp", cf[:], cf_d, writes=["cf"])
        ptb = R1[:, 0:2 * NS * NPG].bitcast(I32)
        ptf = R1[:, 2 * NS * NPG:4 * NS * NPG].bitcast(F32)
        S.dma("sp", ptb, pt_d.partition_broadcast(128), writes=["ptb"])
        S.op("dve", lambda e: e.tensor_copy(out=ptf, in_=ptb), reads=["ptb"], writes=["ptf"])
        S.op("dve", lambda e: e.tensor_scalar(out=ptf, in0=ptf, scalar1=128.0, scalar2=cf[:, 256 + 2 * NH:256 + 2 * NH + 1], op0=ALU.mult, op1=ALU.add),
             reads=["ptf", "cf"], writes=["ptf"])
        S.op("dve", lambda e: e.tensor_copy(out=idx[:], in_=ptf), reads=["ptf"], writes=["idx"])
        S.dma("sp", bfn[:], bfT_d, writes=["bfn"])
        S.op("dve", lambda e: e.tensor_scalar(out=bfn[:], in0=bfn[:], scalar1=-1.0, scalar2=None, op0=ALU.mult), reads=["bfn"], writes=["bfn"])
        for l in range(depth):
            src = vecs[:, l * LV + V_LAM:l * LV + V_LAM + KC]
            dst = lsc[:, l * KC:(l + 1) * KC]
            S.op("act", lambda e, s=src, d=dst: e.activation(out=d, in_=s, func=AF.Exp, scale=-1.0), reads=["vecs"], writes=["lsc"])
            S.op("act", lambda e, d=dst: e.activation(out=d, in_=d, func=AF.Ln, bias=1.0), reads=["lsc"], writes=["lsc"])
            S.op("dve", lambda e, d=dst: e.tensor_scalar(out=d, in0=d, scalar1=-8.0, scalar2=None, op0=ALU.mult), reads=["lsc"], writes=["lsc"])

        def vcol(l, base, k):
            c = (l * LV if l is not None else 0) + base + k
            return vecs[:, c:c + 1]

        def rmsnorm(gl, gbase, fp32_out=None):
            for t, (c0, w) in enumerate(TILES):
                ps, pk = ps_gen()
                for k in range(KC):
                    s_ = sqt[:, 0, 0:w]
                    sk = "sqt0"
                    S.op("act", lambda e, s_=s_, k=k, c0=c0, w=w: e.activation(out=s_, in_=xT[:, k, c0:c0 + w], func=AF.Square), reads=[xk(t, k)], writes=[sk])
                    S.op("pe", lambda e, s_=s_, k=k, ps=ps, w=w: e.matmul(ps[:, 0:w], lhsT=onesb, rhs=s_, start=(k == 0), stop=(k == KC - 1)),
                         reads=[sk, "cst"], writes=[pk])
                rk = "rstd%d" % t
                S.op("act", lambda e, ps=ps, c0=c0, w=w: e.activation(out=rstd[:, c0:c0 + w], in_=ps[:, 0:w], func=AF.Sqrt, scale=1.0 / D, bias=EPS), reads=[pk], writes=[rk])
                S.op("dve", lambda e, c0=c0, w=w: e.reciprocal(out=rstd[:, c0:c0 + w], in_=rstd[:, c0:c0 + w]), reads=[rk], writes=[rk])
                for k in range(KC):
                    g = vcol(gl, gbase, k)
                    if fp32_out is None:
                        S.op("dve", lambda e, k=k, c0=c0, w=w, g=g: e.scalar_tensor_tensor(
                            out=xnT[:, k, c0:c0 + w], in0=xT[:, k, c0:c0 + w], scalar=g, in1=rstd[:, c0:c0 + w], op0=ALU.mult, op1=ALU.mult),
                            reads=[xk(t, k), rk, "vecs"], writes=[xnk(t, k)])
                    else:
                        fp32_out(t, k, c0, w, g, rk)

        def evac_add_x(ps, pk, t, m, c0, w):
            S.op("dve", lambda e: e.tensor_tensor(out=xT[:, m, c0:c0 + w], in0=ps[:, 0:w], in1=xT[:, m, c0:c0 + w], op=ALU.add),
                 reads=[pk, xk(t, m)], writes=[xk(t, m)])

        def decode_gen(l, attnT):
            w_in_l = w_in_d[l]
            f32v = lambda off, n: R1[:, off:off + 2 * n].bitcast(F32)
            rsb = rstd[:, :].bitcast(BF16)
            pg = [f32v(i * 1024, 512) for i in range(3)] + [rsb[:, i * 1024:(i + 1) * 1024].bitcast(F32) for i in range(3)]
            dX = f32v(3072, 512)
            dXb = [R1[:, 3072:3584], R1[:, 3584:4096]]
            dSP = f32v(4096, 512)
            dlat = f32v(5120, 520)
            dqs = f32v(6160, 512)
            dvwn = R1[:, 7184:7696]
            dm = f32v(7696, 64)
            lfs, pnew, snew = dm[0:NS, 0:8], dm[0:NS, 8:16], dm[0:NS, 16:24]
            lfnb, rs, rden = dm[:, 24:32], dm[:, 32:40], dm[0:1, 40:48]
            SP3 = dSP.rearrange("p (j h) -> p j h", h=NH)
            X3 = dX.rearrange("p (j h) -> p j h", h=NH)
            lat3 = dlat[:, 0:512].rearrange("p (j h) -> p j h", h=NH)
            xs_keys = xnkeys(4)
            dsts = [dqs[0:NS, :], pg[0][0:NS, :], pg[1][0:NS, :]]
            dkeys = ["dqs", "pg0", "pg1"]
            for i, c0 in enumerate((OQ, OK_, OV)):
                wsl, kw = slab([(0, KC, 512, wview(w_in_l, c0, 512))])
                wsl3 = w3(wsl, KC)
                ps, pk = ps_gen()
                proj(ps[0:NS, :], pk, lambda k: xnT[:, k, T:T + NS], KC, lambda k: wsl3[:, k, :], xs_keys, kw)
                S.op("dve", lambda e, ps=ps, i=i: e.tensor_copy(out=dsts[i], in_=ps[0:NS, :]), reads=[pk], writes=[dkeys[i]])
            wsl, kw = slab([(0, KC, 8, wview(w_in_l, OFL, 8))])
            wsl3 = w3(wsl, KC, 8)
            ps, pk = ps_gen()
            proj(ps[0:NS, 0:NH], pk, lambda k: xnT[:, k, T:T + NS], KC, lambda k: wsl3[:, k, :], xs_keys, kw)
            S.op("dve", lambda e, ps=ps: e.tensor_tensor(out=lfs, in0=ps[0:NS, 0:NH], in1=bfrow[0:NS, l * NH:(l + 1) * NH], op=ALU.add), reads=[pk, "cf"], writes=["dmisc"])
            S.op("act", lambda e: e.activation(out=lfs, in_=lfs, func=AF.Exp, scale=-1.0), reads=["dmisc"], writes=["dmisc"])
            S.op("act", lambda e: e.activation(out=lfs, in_=lfs, func=AF.Ln, bias=1.0), reads=["dmisc"], writes=["dmisc"])
            S.op("dve", lambda e: e.tensor_scalar(out=lfs, in0=lfs, scalar1=-1.0, scalar2=None, op0=ALU.mult), reads=["dmisc"], writes=["dmisc"])
            S.op("dve", lambda e: e.tensor_tensor(out=pg[2][0:NS, :], in0=dqs[0:NS, :], in1=pg[0][0:NS, :], op=ALU.mult), reads=["dqs", "pg0"], writes=["pg2"])
            S.op("dve", lambda e: e.tensor_reduce(out=snew, in_=pg[2][0:NS, :].rearrange("p (h d) -> p h d", h=NH), axis=AX.X, op=ALU.add), reads=["pg2"], writes=["dmisc"])
            S.op("act", lambda e: e.activation(out=pnew, in_=snew, func=AF.Exp, scale=0.125), reads=["dmisc"], writes=["dmisc"])
            S.op("dve", lambda e: e.tensor_tensor(out=dvwn[0:NS, :].rearrange("p (h d) -> p h d", h=NH), in0=pg[1][0:NS, :].rearrange("p (h d) -> p h d", h=NH),
                                                  in1=pnew.unsqueeze(2).broadcast_to([NS, NH, DH]), op=ALU.mult), reads=["pg1", "dmisc"], writes=["dvwn"])
            yield

            def page_dma(dram4, b, j, dst, key, deps=()):
                src2d = dram4.rearrange("l n p f -> (l n p) f")
                col = b * NPG + j
                eoff = l * NPOOL * 128 * int(dram4.shape[3])

                def fn(e):
                    return e.indirect_dma_start(out=dst, out_offset=None, in_=src2d,
                                                in_offset=bass.IndirectOffsetOnAxis(ap=idx[:, col:col + 1], axis=0),
                                                element_offset=eoff)
                return S.dma_fn("pool", fn, reads=["idx"], writes=[key], deps=deps)

            for b in range(NS):
                S.op("dve", lambda e: e.tensor_scalar(out=pg[2][0:NS, :], in0=dqs[0:NS, :], scalar1=identf[0:NS, b:b + 1], scalar2=None, op0=ALU.mult),
                     reads=["dqs", "identf"], writes=["pg2"])
                ps, pk = ps_gen()
                S.op("pe", lambda e, ps=ps: e.matmul(ps[:, :], lhsT=onesf[0:NS, :], rhs=pg[2][0:NS, :], start=True, stop=True), reads=["pg2", "cf"], writes=[pk])
                S.op("act", lambda e, ps=ps: e.activation(out=dlat[:, 0:512], in_=ps[:, :], func=AF.Identity, scale=0.125), reads=[pk], writes=["dlat"])
                S.op("dve", lambda e: e.tensor_scalar(out=dm[0:NS, 48:56], in0=lfs, scalar1=identf[0:NS, b:b + 1], scalar2=None, op0=ALU.mult), reads=["dmisc", "identf"], writes=["dmisc"])
                ps, pk = ps_gen()
                S.op("pe", lambda e, ps=ps: e.matmul(ps[:, 0:NH], lhsT=onesf[0:NS, :], rhs=dm[0:NS, 48:56], start=True, stop=True), reads=["dmisc", "cf"], writes=[pk])
                S.op("dve", lambda e, ps=ps: e.tensor_copy(out=lfnb, in_=ps[:, 0:NH]), reads=[pk], writes=["dmisc"])
                yield
                for j in range(NPG):
                    pi = j % 6
                    page_dma(ck_d, b, j, pg[pi], "pg%d" % pi)
                    S.op("dve", lambda e, pi=pi: e.tensor_tensor(out=dX, in0=pg[pi], in1=dlat[:, 0:512], op=ALU.mult), reads=["pg%d" % pi, "dlat"], writes=["dX"])
                    lastred = S.op("dve", lambda e, j=j: e.tensor_reduce(out=SP3[:, j, :], in_=dX.rearrange("p (h d) -> p h d", h=NH), axis=AX.X, op=ALU.add), reads=["dX"], writes=["dSP"])
                    yield
                lkeys = ["dXl%d" % j for j in range(NPG)]
                for j in range(NPG):
                    page_dma(clf_d, b, j, X3[:, j, :], lkeys[j], deps=[lastred])
                psA, pkA = ps_gen()
                S.op("pe", lambda e, psA=psA: e.matmul(psA[:, :], lhsT=utri, rhs=dX, start=True, stop=True), reads=["dX", "cf"] + lkeys, writes=[pkA])
                psB, pkB = ps_gen()
                S.op("pe", lambda e, psB=psB: e.matmul(psB[:, :], lhsT=onesf, rhs=dX, start=True, stop=True), reads=["dX", "cf"] + lkeys, writes=[pkB])
                S.op("dve", lambda e, psA=psA: e.tensor_tensor(out=dSP, in0=psA[:, :], in1=dSP, op=ALU.add), reads=[pkA, "dSP"], writes=["dSP"])
                S.op("dve", lambda e, psB=psB: e.tensor_copy(out=dlat[:, 0:512], in_=psB[:, :]), reads=[pkB], writes=["dlat"])
                for h in range(NH):
                    init = 0.0
                    S.op("dve", lambda e, h=h: e.tensor_tensor_scan(out=X3[:, :, h], data0=onesf[:, 0:NPG], data1=lat3[:, :, h], initial=0.0, op0=ALU.mult, op1=ALU.add),
                         reads=["dlat", "cf"], writes=["dX"])
                S.op("dve", lambda e: e.tensor_tensor(out=lat3, in0=X3[:, NPG - 1:NPG, :].broadcast_to([128, NPG, NH]), in1=X3, op=ALU.subtract), reads=["dX"], writes=["dlat"])
                S.op("dve", lambda e: e.tensor_tensor(out=dSP, in0=dSP, in1=dlat[:, 0:512], op=ALU.add), reads=["dSP", "dlat"], writes=["dSP"])
                S.op("dve", lambda e: e.tensor_tensor(out=SP3, in0=SP3, in1=lfnb.unsqueeze(1).broadcast_to([128, NPG, NH]), op=ALU.add), reads=["dSP", "dmisc"], writes=["dSP"])
                S.op("act", lambda e: e.activation(out=dSP, in_=dSP, func=AF.Exp), reads=["dSP"], writes=["dSP"])
                S.op("dve", lambda e: e.tensor_reduce(out=rs, in_=dSP.rearrange("p (j h) -> p h j", h=NH), axis=AX.X, op=ALU.add), reads=["dSP"], writes=["dmisc"])
                yield
                acc, ak = psb[3], "ps3"
                for j in range(NPG):
                    pi = j % 6
                    page_dma(cv_d, b, j, pg[pi], "pg%d" % pi)
                    vw = dXb[j % 2]
                    S.op("dve", lambda e, pi=pi, vw=vw, j=j: e.tensor_tensor(out=vw.rearrange("p (h d) -> p h d", h=NH), in0=pg[pi].rearrange("p (h d) -> p h d", h=NH),
                                                                           in1=SP3[:, j, :].unsqueeze(2).broadcast_to([128, NH, DH]), op=ALU.mult),
                         reads=["pg%d" % pi, "dSP"], writes=["dXb%d" % (j % 2), "dX"])
                    S.op("pe", lambda e, vw=vw, j=j: e.matmul(acc[0:1, :], lhsT=onesb[:, 0:1], rhs=vw, start=(j == 0), stop=False), reads=["dXb%d" % (j % 2), "cst"], writes=[ak])
                    yield
                S.op("pe", lambda e: e.matmul(acc[0:1, :], lhsT=ident[0:NS, b:b + 1], rhs=dvwn[0:NS, :], start=False, stop=True), reads=["dvwn", "cst"], writes=[ak])
                psD, pkD = ps_gen()
                S.op("pe", lambda e, psD=psD: e.matmul(psD[0:1, 0:NH], lhsT=onesf[:, 0:1], rhs=rs, start=True, stop=False), reads=["dmisc", "cf"], writes=[pkD])
                S.op("pe", lambda e, psD=psD: e.matmul(psD[0:1, 0:NH], lhsT=identf[0:NS, b:b + 1], rhs=pnew, start=False, stop=True), reads=["dmisc", "identf"], writes=[pkD])
                S.op("dve", lambda e, psD=psD: e.reciprocal(out=rden, in_=psD[0:1, 0:NH]), reads=[pkD], writes=["dmisc"])
                orow = pg[0][0:1, :]
                S.op("dve", lambda e: e.tensor_tensor(out=orow.rearrange("p (h d) -> p h d", h=NH), in0=acc[0:1, :].rearrange("p (h d) -> p h d", h=NH),
                                                      in1=rden.unsqueeze(2).broadcast_to([1, NH, DH]), op=ALU.mult), reads=[ak, "dmisc"], writes=["pg0"])
                psC, pkC = ps_gen()
                for c in range(4):
                    S.op("pe", lambda e, psC=psC, c=c: e.matmul(psC[:, c:c + 1], lhsT=orow[:, c * 128:(c + 1) * 128], rhs=identf[0:1, 0:1], start=True, stop=True),
                         reads=["pg0", "identf"], writes=[pkC])
                S.op("dve", lambda e, psC=psC: e.tensor_copy(out=attnT[:, :, T + b], in_=psC[:, 0:4]), reads=[pkC], writes=["attnS"])
                yield

        for l in range(depth):
            w_in_l = w_in_d[l]
            S.dma("pool", wgate[:, 0], w_rg_d[l].rearrange("n c d -> c n d"), writes=["wgate"])
            S.dma("pool", wgate[:, 1], w_ig_d[l].rearrange("n c d -> c n d"), writes=["wgate"])
            rmsnorm(l, V_GMIX)
            fence(R2KEYS + R1KEYS)

            lfT = r2f(8512, NTOK)[0:NH]
            cT = r2f(12672, T)[0:NH]
            ones8 = r2f(16768, 512)[0:NH]
            S.op("dve", lambda e: e.memset(ones8, 1.0), writes=["ones8"])
            wfl, kfl = slab([(0, KC, 8, wview(w_in_l, OFL, 8))])
            wfl3 = w3(wfl, KC, 8)
            for t, (c0, w) in enumerate(TILES):
                ps, pk = ps_gen()
                proj(ps[0:NH, 0:w], pk, lambda k: wfl3[:, k, :], KC, lambda k: xnT[:, k, c0:c0 + w], xnkeys(t), kfl)
                S.op("act", lambda e, ps=ps, c0=c0, w=w: e.activation(out=lfT[:, c0:c0 + w], in_=ps[0:NH, 0:w], func=AF.Exp, scale=-1.0, bias=bfn[:, l:l + 1]),
                     reads=[pk, "bfn"], writes=["lfT"])
                S.op("act", lambda e, c0=c0, w=w: e.activation(out=lfT[:, c0:c0 + w], in_=lfT[:, c0:c0 + w], func=AF.Ln, bias=1.0), reads=["lfT"], writes=["lfT"])
                S.op("dve", lambda e, c0=c0, w=w: e.tensor_scalar(out=lfT[:, c0:c0 + w], in0=lfT[:, c0:c0 + w], scalar1=-1.0, scalar2=None, op0=ALU.mult),
                     reads=["lfT"], writes=["lfT"])
                if t < 4:
                    init = 0.0 if t == 0 else cT[:, c0 - 1:c0]
                    S.op("dve", lambda e, c0=c0, w=w, init=init: e.tensor_tensor_scan(out=cT[:, c0:c0 + w], data0=ones8[:, 0:w], data1=lfT[:, c0:c0 + w],
                                                                                     initial=init, op0=ALU.mult, op1=ALU.add),
                         reads=["lfT", "ones8", "cT"], writes=["cT"])
            finals.append(S.dma("sp", lfT_o[l], lfT, reads=["lfT"]))
            S.op("act", lambda e: e.activation(out=c8b[:], in_=cT, func=AF.Identity, scale=8.0), reads=["cT"], writes=["c8b"])
            for g4 in range(4):
                ps, pk = ps_gen()
                for j in range(4):
                    tt = g4 * 4 + j
                    S.op("pe", lambda e, ps=ps, j=j, tt=tt: e.matmul(ps[:, j * 8:(j + 1) * 8], lhsT=cT[:, tt * 128:(tt + 1) * 128], rhs=identf[0:NH, 0:NH], start=True, stop=True),
                         reads=["cT", "identf"], writes=[pk])
                S.op("dve", lambda e, ps=ps, g4=g4: e.tensor_scalar(out=negc[:, g4 * 4:(g4 + 1) * 4, :], in0=ps[:, 0:32].rearrange("p (j h) -> p j h", j=4),
                                                                    scalar1=-1.0, scalar2=None, op0=ALU.mult), reads=[pk], writes=["negc"])
            fence(R2KEYS)
            if stop_after == "logf":
                S.emit(finals)
                return nc

            qTm = R2[:, 0:2080]
            kTm = R2[:, 2080:4160]
            Vp = R2[:, 4160:4160 + 17 * 256].rearrange("p (t h c) -> p t h c", t=17, h=2)
            attnT = R2[:, 8512:8512 + 4 * NTOK].rearrange("p (c n) -> p c n", c=4)
            PT = [R2[:, 16832 + i * 512:16832 + (i + 1) * 512] for i in range(3)]
            S.op("dve", lambda e: e.memset(R2[:, 4160:4160 + 17 * 256], 0.0), writes=["Vp"])
            S.op("dve", lambda e: e.memset(qTm[64:128, :], 0.0), writes=["qTs"])
            S.op("dve", lambda e: e.memset(kTm[64:128, :], 0.0), writes=["kTs"])
            S.dma("sp", qTm[64:72, 0:T], c8b[:, :], reads=["c8b"], writes=["qTs"])
            psrot[0] = [0, 1, 2]
            fence(DKEYS + RSKEYS)
            dg = decode_gen(l, attnT)

            def dstep(n=1):
                for _ in range(n):
                    next(dg, None)
            dstep()
            stgi = [0]

            def stage_out(ps_ap, pk, np_, ncol, dram_ap, after):
                si = stgi[0] % 2
                stgi[0] += 1
                S.op("act", lambda e: e.copy(out=stg[si][0:np_, 0:ncol], in_=ps_ap), reads=[pk], writes=["stg%d" % si], deps=[after])
                finals.append(S.dma("sp", dram_ap, stg[si][0:np_, 0:ncol], reads=["stg%d" % si]))

            def qkv_slab(hp_):
                return slab([(0, KC, 128, wview(w_in_l, OQ + hp_ * 128, 128)),
                             (1024, KC, 128, wview(w_in_l, OK_ + hp_ * 128, 128)),
                             (2048, KC, 128, wview(w_in_l, OV + hp_ * 128, 128))])
            nxt = qkv_slab(0)
            for hp in range(4):
                wq, kq = nxt
                if hp < 3:
                    nxt = qkv_slab(hp + 1)
                wq3, wk3, wv3 = w3(wq, KC, 128, 0), w3(wq, KC, 128, 1024), w3(wq, KC, 128, 2048)
                for tt in range(17):
                    r0, nr = tt * 128, (128 if tt < 16 else NS)
                    t = min(tt // 4, 4)
                    ps, pk = ps_gen()
                    proj(ps[0:nr, 0:128], pk, lambda k: xnT[:, k, r0:r0 + nr], KC, lambda k: wv3[:, k, :], xnkeys(t), kq)
                    S.op("act", lambda e, ps=ps, tt=tt, nr=nr: e.copy(out=Vp[0:nr, tt, 0, 0:64], in_=ps[0:nr, 0:64]), reads=[pk], writes=["Vp"])
                    o_ = S.op("act", lambda e, ps=ps, tt=tt, nr=nr: e.copy(out=Vp[0:nr, tt, 1, 64:128], in_=ps[0:nr, 64:128]), reads=[pk], writes=["Vp"])
                    stage_out(ps[0:nr, 0:128], pk, nr, 128, v_o[l, r0:r0 + nr, hp * 128:(hp + 1) * 128], o_)
                    dstep()
                for hh in range(2):
                    h = 2 * hp + hh
                    S.dma("pool", kTm[64:72, 0:T], oh_d[h], writes=["kTs", "kTb"])
                    for t, (c0, w) in enumerate(TILES):
                        ps, pk = ps_gen()
                        proj(ps[0:64, 0:w], pk, lambda k: wq3[:, k, hh * 64:(hh + 1) * 64], KC, lambda k: xnT[:, k, c0:c0 + w], xnkeys(t), kq)
                        S.op("act", lambda e, ps=ps, c0=c0, w=w: e.copy(out=qTm[0:64, c0:c0 + w], in_=ps[0:64, 0:w]), reads=[pk], writes=["qT"])
                        ps, pk = ps_gen()
                        proj(ps[0:64, 0:w], pk, lambda k: wk3[:, k, hh * 64:(hh + 1) * 64], KC, lambda k: xnT[:, k, c0:c0 + w], xnkeys(t), kq)
                        o_ = S.op("act", lambda e, ps=ps, c0=c0, w=w: e.copy(out=kTm[0:64, c0:c0 + w], in_=ps[0:64, 0:w]), reads=[pk], writes=["kTb"])
                        stage_out(ps[0:64, 0:w], pk, 64, w, kT_o[l, h * 64:(h + 1) * 64, c0:c0 + w], o_)
                        dstep()
                    for qi in range(4):
                        q0 = qi * 512
                        pO, pS = psb[6], psb[7]
                        nkj = 4 * qi + 4
                        pend = None
                        for kj in range(nkj):
                            r = kj - 4 * qi
                            cs = max(r, 0) * 128
                            bank = 4 + (kj % 2)
                            pst, pkt = psb[bank], "ps%d" % bank
                            S.op("pe", lambda e, pst=pst, kj=kj, cs=cs, r=r: e.matmul(
                                pst[:, cs:512], lhsT=kTm[:, kj * 128:(kj + 1) * 128], rhs=qTm[:, q0 + cs:q0 + 512], start=True, stop=(r < 0)),
                                reads=["qT", "kTb", "qTs", "kTs"], writes=[pkt])
                            if r >= 0:
                                S.op("pe", lambda e, pst=pst, cs=cs: e.matmul(pst[:, cs:cs + 128], lhsT=ident, rhs=mneg, start=False, stop=True),
                                     reads=["cst"], writes=[pkt])
                            pt_i = kj % 3
                            S.op("act", lambda e, pst=pst, pt_i=pt_i, kj=kj, h=h, cs=cs: e.activation(
                                out=PT[pt_i][:, cs:512], in_=pst[:, cs:512], func=AF.Exp, scale=0.125, bias=negc[:, kj, h:h + 1]),
                                reads=[pkt, "negc"], writes=["PT%d" % pt_i])
                            dstep()
                            cur = (pt_i, kj, hh, cs, kj)
                            if pend is not None:
                                _pv(S, pend, PT, Vp, opad, pO, pS, nkj)
                            pend = cur
                        _pv(S, pend, PT, Vp, opad, pO, pS, nkj)
                        si = stgi[0] % 2
                        stgi[0] += 1
                        rc = stg[si]
                        lo, hi = hh * 64, (hh + 1) * 64
                        S.op("dve", lambda e, rc=rc, lo=lo, hi=hi: e.reciprocal(out=rc[lo:hi, :], in_=pS[lo:hi, :]), reads=["ps7"], writes=["stg%d" % si])
                        S.op("dve", lambda e, rc=rc, q0=q0, hp=hp, lo=lo, hi=hi: e.tensor_tensor(out=attnT[lo:hi, hp, q0:q0 + 512], in0=pO[lo:hi, :], in1=rc[lo:hi, :], op=ALU.mult),
                             reads=["ps6", "stg%d" % si], writes=["attn%d" % hp])
            for _ in dg:
                pass
            psrot[0] = list(range(8))
            fence(["qT", "kTb", "qTs", "kTs", "Vp", "PT0", "PT1", "PT2", "t1", "t2", "t3", "xcb", "sgt", "sga", "tmpm"] + R1KEYS)

            if stop_after == "attn":
                S.emit(finals)
                return nc
            yTt = w3(R1, KC, 512, 0)
            mrg = w3(R1, KC, 512, 4096)
            t1, t2, t3 = [r2f(i * 1056, 528) for i in range(3)]
            xcb = R2[:, 3168:3680]
            sgt = r2f(3680, 512)
            sga = r2f(4704, 512)
            tmpm = r2f(5728, 512)
            S.op("dve", lambda e: e.memset(carry[:], 0.0), writes=["carry"])
            S.op("dve", lambda e: e.memset(hcar[:], 0.0), writes=["hcar"])
            S.dma("sp", prevS[:], scT_d[l].rearrange("(k p) j b -> p k j b", p=128), writes=["prevS"])
            S.dma("sp", h0S[:], shT_d[l].rearrange("(k p) b -> p k b", p=128), writes=["h0S"])
            for t, (c0, w) in enumerate(TILES):
                samp = (t == 4)
                for half in range(2):
                    wxr, kxr = slab([(0, KC, 512, wview(w_in_l, OXR + half * 512, 512))])
                    wgr, kgr = slab([(0, KC, 512, wview(w_in_l, OGR + half * 512, 512))])
                    wxr3, wgr3 = w3(wxr, KC), w3(wgr, KC)
                    for nn in range(4):
                        n = half * 4 + nn
                        ps, pk = ps_gen()
                        proj(ps[:, 0:w], pk, lambda k: wxr3[:, k, nn * 128:(nn + 1) * 128], KC, lambda k: xnT[:, k, c0:c0 + w], xnkeys(t), kxr)
                        cw = [vcol(l, V_CW + j * 8, n) for j in range(4)]
                        cb = vcol(l, V_CB, n)
                        if not samp:
                            S.op("dve", lambda e, n=n: e.tensor_copy(out=t1[:, 0:3], in_=carry[:, n, :]), reads=["carry"], writes=["t1"])
                            S.op("act", lambda e, ps=ps, w=w: e.copy(out=t1[:, 3:3 + w], in_=ps[:, 0:w]), reads=[pk], writes=["t1"])
                            S.op("dve", lambda e, n=n, w=w: e.tensor_copy(out=carry[:, n, :], in_=t1[:, w:w + 3]), reads=["t1"], writes=["carry"])
                            S.op("dve", lambda e, w=w, cw=cw, cb=cb: e.tensor_scalar(out=t2[:, 0:w], in0=t1[:, 3:3 + w], scalar1=cw[3], scalar2=cb, op0=ALU.mult, op1=ALU.add),
                                 reads=["t1", "vecs"], writes=["t2"])
                            for j in range(3):
                                S.op("dve", lambda e, j=j, w=w, cw=cw: e.scalar_tensor_tensor(out=t2[:, 0:w], in0=t1[:, j:j + w], scalar=cw[j], in1=t2[:, 0:w], op0=ALU.mult, op1=ALU.add),
                                     reads=["t1", "t2", "vecs"], writes=["t2"])
                        else:
                            S.op("act", lambda e, ps=ps, w=w: e.copy(out=t1[:, 0:w], in_=ps[:, 0:w]), reads=[pk], writes=["t1"])
                            S.op("dve", lambda e, w=w, cw=cw, cb=cb: e.tensor_scalar(out=t2[:, 0:w], in0=t1[:, 0:w], scalar1=cw[3], scalar2=cb, op0=ALU.mult, op1=ALU.add),
                                 reads=["t1", "vecs"], writes=["t2"])
                            for j in range(3):
                                S.op("dve", lambda e, j=j, n=n, w=w, cw=cw: e.scalar_tensor_tensor(out=t2[:, 0:w], in0=prevS[:, n, j, :], scalar=cw[j], in1=t2[:, 0:w], op0=ALU.mult, op1=ALU.add),
                                     reads=["prevS", "t2", "vecs"], writes=["t2"])
                            S.op("dve", lambda e, n=n, w=w: e.tensor_copy(out=xrS[:, n, :], in_=t1[:, 0:w]), reads=["t1"], writes=["xrS"])
                        S.op("dve", lambda e, w=w: e.tensor_copy(out=xcb[:, 0:w], in_=t2[:, 0:w]), reads=["t2"], writes=["xcb"])
                        psr, pkr = ps_gen()
                        S.op("pe", lambda e, psr=psr, n=n, w=w: e.matmul(psr[:, 0:w], lhsT=wgate[:, 0, n, :], rhs=xcb[:, 0:w], start=True, stop=True), reads=["wgate", "xcb"], writes=[pkr])
                        psi_, pki = ps_gen()
                        S.op("pe", lambda e, psi_=psi_, n=n, w=w: e.matmul(psi_[:, 0:w], lhsT=wgate[:, 1, n, :], rhs=xcb[:, 0:w], start=True, stop=True), reads=["wgate", "xcb"], writes=[pki])
                        S.op("act", lambda e, psr=psr, n=n, w=w: e.activation(out=t1[:, 0:w], in_=psr[:, 0:w], func=AF.Sigmoid, bias=vcol(l, V_BRG, n)), reads=[pkr, "vecs"], writes=["t1"])
                        S.op("act", lambda e, psi_=psi_, n=n, w=w: e.activation(out=t3[:, 0:w], in_=psi_[:, 0:w], func=AF.Sigmoid, bias=vcol(l, V_BIG, n)), reads=[pki, "vecs"], writes=["t3"])
                        S.op("act", lambda e, n=n, w=w: e.activation(out=t1[:, 0:w], in_=t1[:, 0:w], func=AF.Exp, scale=lsc[:, l * KC + n:l * KC + n + 1]), reads=["t1", "lsc"], writes=["t1"])
                        S.op("dve", lambda e, w=w: e.tensor_tensor(out=t3[:, 0:w], in0=t3[:, 0:w], in1=t2[:, 0:w], op=ALU.mult), reads=["t3", "t2"], writes=["t3"])
                        S.op("dve", lambda e, w=w: e.tensor_tensor(out=t2[:, 0:w], in0=t1[:, 0:w], in1=t1[:, 0:w], op=ALU.mult), reads=["t1"], writes=["t2"])
                        S.op("act", lambda e, w=w: e.activation(out=t2[:, 0:w], in_=t2[:, 0:w], func=AF.Sqrt, scale=-1.0, bias=1.0), reads=["t2"], writes=["t2"])
                        S.op("dve", lambda e, w=w: e.tensor_tensor(out=t3[:, 0:w], in0=t3[:, 0:w], in1=t2[:, 0:w], op=ALU.mult), reads=["t3", "t2"], writes=["t3"])
                        if not samp:
                            S.op("dve", lambda e, n=n, w=w: e.tensor_tensor_scan(out=t2[:, 0:w], data0=t1[:, 0:w], data1=t3[:, 0:w], initial=hcar[:, n:n + 1], op0=ALU.mult, op1=ALU.add),
                                 reads=["t1", "t3", "hcar"], writes=["t2"])
                            S.op("dve", lambda e, n=n, w=w: e.tensor_copy(out=hcar[:, n:n + 1], in_=t2[:, w - 1:w]), reads=["t2"], writes=["hcar"])
                        else:
                            S.op("dve", lambda e, n=n, w=w: e.tensor_tensor(out=t2[:, 0:w], in0=t1[:, 0:w], in1=h0S[:, n, :], op=ALU.mult), reads=["t1", "h0S"], writes=["t2"])
                            S.op("dve", lambda e, w=w: e.tensor_tensor(out=t2[:, 0:w], in0=t2[:, 0:w], in1=t3[:, 0:w], op=ALU.add), reads=["t2", "t3"], writes=["t2"])
                            S.op("dve", lambda e, n=n, w=w: e.tensor_copy(out=hS[:, n, 1:1 + w], in_=t2[:, 0:w]), reads=["t2"], writes=["hS"])
                        psg, pkg = ps_gen()
                        proj(psg[:, 0:w], pkg, lambda k: wgr3[:, k, nn * 128:(nn + 1) * 128], KC, lambda k: xnT[:, k, c0:c0 + w], xnkeys(t), kgr)
                        S.op("act", lambda e, psg=psg, w=w: e.activation(out=t3[:, 0:w], in_=psg[:, 0:w], func=AF.Gelu_apprx_tanh), reads=[pkg], writes=["t3"])
                        S.op("dve", lambda e, n=n, w=w: e.tensor_tensor(out=yTt[:, n, 0:w], in0=t2[:, 0:w], in1=t3[:, 0:w], op=ALU.mult), reads=["t2", "t3"], writes=["y%d" % n])
                for half in range(2):
                    wb, kb = slab([(0, KC, 512, wview(w_b_d[l], half * 512, 512))])
                    wgb, kgb = slab([(0, KC, 512, wview(w_in_l, OGB + half * 512, 512))])
                    wb3, wgb3 = w3(wb, KC), w3(wgb, KC)
                    for mm in range(4):
                        m = half * 4 + mm
                        psB, pkB = ps_gen()
                        proj(psB[:, 0:w], pkB, lambda k: wb3[:, k, mm * 128:(mm + 1) * 128], KC, lambda k: yTt[:, k, 0:w], ["y%d" % k for k in range(KC)], kb)
                        psG, pkG = ps_gen()
                        proj(psG[:, 0:w], pkG, lambda k: wgb3[:, k, mm * 128:(mm + 1) * 128], KC, lambda k: xnT[:, k, c0:c0 + w], xnkeys(t), kgb)
                        S.op("act", lambda e, psG=psG, w=w: e.activation(out=sgt[:, 0:w], in_=psG[:, 0:w], func=AF.Sigmoid), reads=[pkG], writes=["sgt"])
                        S.op("dve", lambda e, psB=psB, m=m, w=w: e.tensor_tensor(out=mrg[:, m, 0:w], in0=psB[:, 0:w], in1=sgt[:, 0:w], op=ALU.mult), reads=[pkB, "sgt"], writes=["mg%d" % m])
                for half in range(2):
                    wa, ka = slab([(0, 4, 512, wview(w_a_d[l], half * 512, 512))])
                    wga, kga = slab([(0, KC, 512, wview(w_in_l, OGA + half * 512, 512))])
                    wa3, wga3 = w3(wa, 4), w3(wga, KC)
                    for mm in range(4):
                        m = half * 4 + mm
                        psA, pkA = ps_gen()
                        akeys = ["attn%d" % k for k in range(4)] if not samp else ["attnS"] * 4
                        proj(psA[:, 0:w], pkA, lambda k: wa3[:, k, mm * 128:(mm + 1) * 128], 4, lambda k: attnT[:, k, c0:c0 + w], akeys, ka)
                        psG, pkG = ps_gen()
                        proj(psG[:, 0:w], pkG, lambda k: wga3[:, k, mm * 128:(mm + 1) * 128], KC, lambda k: xnT[:, k, c0:c0 + w], xnkeys(t), kga)
                        S.op("act", lambda e, psG=psG, w=w: e.activation(out=sga[:, 0:w], in_=psG[:, 0:w], func=AF.Sigmoid), reads=[pkG], writes=["sga"])
                        S.op("dve", lambda e, psA=psA, w=w: e.tensor_tensor(out=tmpm[:, 0:w], in0=psA[:, 0:w], in1=sga[:, 0:w], op=ALU.mult), reads=[pkA, "sga"], writes=["tmpm"])
                        S.op("dve", lambda e, m=m, w=w: e.tensor_tensor(out=mrg[:, m, 0:w], in0=tmpm[:, 0:w], in1=mrg[:, m, 0:w], op=ALU.add), reads=["tmpm", "mg%d" % m], writes=["mg%d" % m])
                for half in range(2):
                    wo, ko = slab([(0, KC, 512, wview(w_o_d[l], half * 512, 512))])
                    wo3 = w3(wo, KC)
                    for mm in range(4):
                        m = half * 4 + mm
                        ps, pk = ps_gen()
                        proj(ps[:, 0:w], pk, lambda k: wo3[:, k, mm * 128:(mm + 1) * 128], KC, lambda k: mrg[:, k, 0:w], ["mg%d" % k for k in range(KC)], ko)
                        evac_add_x(ps, pk, t, m, c0, w)
            S.op("dve", lambda e: e.tensor_copy(out=hS[:, :, 0:1], in_=hcar[:].unsqueeze(2)), reads=["hcar"], writes=["hS"])
            finals.append(S.dma("sp", cvT_o[l].rearrange("(k p) j -> p k j", p=128), carry[:], reads=["carry"]))
            cvo = cvS_o[l].rearrange("(k p) j b -> p k j b", p=128)
            finals.append(S.dma("sp", cvo[:, :, 0:2, :], prevS[:, :, 1:3, :], reads=["prevS"]))
            finals.append(S.dma("sp", cvo[:, :, 2, :], xrS[:], reads=["xrS"]))
            finals.append(S.dma("sp", hT_o[l].rearrange("(k p) b -> p k b", p=128), hS[:], reads=["hS"]))

            if stop_after == "mix":
                S.emit(finals)
                return nc
            fence(DKEYS + RSKEYS)
            rmsnorm(l, V_GFFN)
            fence(R2KEYS + R1KEYS)
            hT = w3(R1, 4, NTOK)
            sgf = r2f(0, 512)
            tf = r2f(1024, 512)
            Ge = r2f(2048, NTOK)

            def ffn_pass(wg2d, wu2d, wd2d, gate):
                for fg in range(FCH // 4):
                    wg, kg = slab([(0, KC, 512, wview(wg2d, fg * 512, 512))])
                    wu, ku = slab([(0, KC, 512, wview(wu2d, fg * 512, 512))])
                    wg3, wu3 = w3(wg, KC), w3(wu, KC)
                    for ff in range(4):
                        for t, (c0, w) in enumerate(TILES):
                            psg, pkg = ps_gen()
                            proj(psg[:, 0:w], pkg, lambda k: wg3[:, k, ff * 128:(ff + 1) * 128], KC, lambda k: xnT[:, k, c0:c0 + w], xnkeys(t), kg)
                            psu, pku = ps_gen()
                            proj(psu[:, 0:w], pku, lambda k: wu3[:, k, ff * 128:(ff + 1) * 128], KC, lambda k: xnT[:, k, c0:c0 + w], xnkeys(t), ku)
                            S.op("act", lambda e, psg=psg, w=w: e.activation(out=sgf[:, 0:w], in_=psg[:, 0:w], func=AF.Silu), reads=[pkg], writes=["sgf"])
                            hk = "h%d_%d" % (ff, t)
                            if not gate:
                                S.op("dve", lambda e, psu=psu, ff=ff, c0=c0, w=w: e.tensor_tensor(out=hT[:, ff, c0:c0 + w], in0=psu[:, 0:w], in1=sgf[:, 0:w], op=ALU.mult),
                                     reads=[pku, "sgf"], writes=[hk])
                            else:
                                S.op("dve", lambda e, psu=psu, w=w: e.tensor_tensor(out=tf[:, 0:w], in0=psu[:, 0:w], in1=sgf[:, 0:w], op=ALU.mult), reads=[pku, "sgf"], writes=["tf"])
                                S.op("dve", lambda e, ff=ff, c0=c0, w=w: e.tensor_tensor(out=hT[:, ff, c0:c0 + w], in0=tf[:, 0:w], in1=Ge[:, c0:c0 + w], op=ALU.mult),
                                     reads=["tf", "Ge"], writes=[hk])
                    wd, kd = slab([(0, 4, 1024, wd2d[fg * 512:(fg + 1) * 512, :].rearrange("(k p) n -> p k n", p=128))])
                    wd3 = w3(wd, 4, 1024)
                    for m in range(KC):
                        for t, (c0, w) in enumerate(TILES):
                            ps, pk = ps_gen()
                            proj(ps[:, 0:w], pk, lambda k: wd3[:, k, m * 128:(m + 1) * 128], 4, lambda k: hT[:, k, c0:c0 + w], ["h%d_%d" % (k, t) for k in range(4)], kd)
                            evac_add_x(ps, pk, t, m, c0, w)

            if l % 2 == 0:
                ffn_pass(dwg_d, dwu_d, dwd_d, False)
            else:
                S.dma("sp", rtw[:], rt_d.rearrange("(k p) n -> p k n", p=128), writes=["rtw"])
                xnf = r2f(2048, NTOK)
                lgS = r2f(6208, NTOK)
                e8f = r2f(10368, 1024)[0:NEXP]
                gwk = r2f(12416, 816).rearrange("p (a b c) -> p a b c", a=6, b=17)
                lgt = r2f(14048, 136).rearrange("p (b c) -> p b c", b=17)
                m12 = r2f(14320, 68).rearrange("p (a b) -> p a b", a=4)
                S.dma("sp", e8f, e8_d, writes=["e8f"])
                for k in range(KC):
                    for t, (c0, w) in enumerate(TILES):
                        S.op("dve", lambda e, k=k, c0=c0, w=w: e.scalar_tensor_tensor(out=xnf[:, c0:c0 + w], in0=xT[:, k, c0:c0 + w], scalar=vcol(l, V_GFFN, k),
                                                                                        in1=rstd[:, c0:c0 + w], op0=ALU.mult, op1=ALU.mult),
                             reads=[xk(t, k), "rstd%d" % t, "vecs"], writes=["xnf%d" % t])
                    for t, (c0, w) in enumerate(TILES):
                        S.op("pe", lambda e, k=k, t=t, c0=c0, w=w: e.matmul(_lg_ps(psb, t)[0:NEXP, 0:w], lhsT=rtw[:, k, :], rhs=xnf[:, c0:c0 + w],
                                                                            start=(k == 0), stop=(k == KC - 1)),
                             reads=["rtw", "xnf%d" % t], writes=[_lg_key(t)])
                for t, (c0, w) in enumerate(TILES):
                    S.op("dve", lambda e, t=t, c0=c0, w=w: e.tensor_copy(out=lgS[0:NEXP, c0:c0 + w], in_=_lg_ps(psb, t)[0:NEXP, 0:w]), reads=[_lg_key(t)], writes=["lgS"])
                for g5 in range(5):
                    ps, pk = ps_gen()
                    nb = 4 if g5 < 4 else 1
                    for j in range(nb):
                        tt = g5 * 4 + j
                        r0, nr = tt * 128, (128 if tt < 16 else NS)
                        S.op("pe", lambda e, ps=ps, j=j, r0=r0, nr=nr: e.matmul(ps[0:nr, j * 8:(j + 1) * 8], lhsT=lgS[0:NEXP, r0:r0 + nr], rhs=identf[0:NEXP, 0:NEXP], start=True, stop=True),
                             reads=["lgS", "identf"], writes=[pk])
                    if g5 == 4:
                        S.op("dve", lambda e: e.memset(lgt[:, 16, :], 0.0), writes=["lgt"])
                        S.op("dve", lambda e, ps=ps: e.tensor_copy(out=lgt[0:NS, 16, :], in_=ps[0:NS, 0:8]), reads=[pk], writes=["lgt"])
                    else:
                        S.op("dve", lambda e, ps=ps, g5=g5: e.tensor_copy(out=lgt[:, g5 * 4:(g5 + 1) * 4, :], in_=ps[:, 0:32].rearrange("p (j h) -> p j h", j=4)), reads=[pk], writes=["lgt"])
                m1, m2, dd, w1 = m12[:, 0], m12[:, 1], m12[:, 2], m12[:, 3]
                eq1, lg2, eq2, g1, g2, gt = [gwk[:, i] for i in range(6)]
                bc = lambda a: a.unsqueeze(2).broadcast_to([128, 17, NEXP])
                S.op("dve", lambda e: e.tensor_reduce(out=m1, in_=lgt, axis=AX.X, op=ALU.max), reads=["lgt"], writes=["m12"])
                S.op("dve", lambda e: e.tensor_tensor(out=eq1, in0=lgt, in1=bc(m1), op=ALU.is_equal), reads=["lgt", "m12"], writes=["gwk"])
                S.op("dve", lambda e: e.scalar_tensor_tensor(out=lg2, in0=eq1, scalar=-1e30, in1=lgt, op0=ALU.mult, op1=ALU.add), reads=["gwk", "lgt"], writes=["gwk"])
                S.op("dve", lambda e: e.tensor_reduce(out=m2, in_=lg2, axis=AX.X, op=ALU.max), reads=["gwk"], writes=["m12"])
                S.op("dve", lambda e: e.tensor_tensor(out=eq2, in0=lg2, in1=bc(m2), op=ALU.is_equal), reads=["gwk", "m12"], writes=["gwk"])
                S.op("dve", lambda e: e.tensor_tensor(out=dd, in0=m2, in1=m1, op=ALU.subtract), reads=["m12"], writes=["m12"])
                S.op("act", lambda e: e.activation(out=dd, in_=dd, func=AF.Exp), reads=["m12"], writes=["m12"])
                S.op("dve", lambda e: e.tensor_scalar(out=w1, in0=dd, scalar1=1.0, scalar2=None, op0=ALU.add), reads=["m12"], writes=["m12"])
                S.op("dve", lambda e: e.reciprocal(out=w1, in_=w1), reads=["m12"], writes=["m12"])
                S.op("dve", lambda e: e.tensor_tensor(out=dd, in0=dd, in1=w1, op=ALU.mult), reads=["m12"], writes=["m12"])
                S.op("dve", lambda e: e.tensor_tensor(out=g1, in0=eq1, in1=bc(w1), op=ALU.mult), reads=["gwk", "m12"], writes=["gwk"])
                S.op("dve", lambda e: e.tensor_tensor(out=g2, in0=eq2, in1=bc(dd), op=ALU.mult), reads=["gwk", "m12"], writes=["gwk"])
                S.op("dve", lambda e: e.tensor_tensor(out=gt, in0=g1, in1=g2, op=ALU.add), reads=["gwk"], writes=["gwk"])
                fence(["lgS", "gatesT"])
                gatesT = lgS[0:NEXP]
                for tt in range(17):
                    r0, nr = tt * 128, (128 if tt < 16 else NS)
                    ps, pk = ps_gen()
                    S.op("pe", lambda e, ps=ps, tt=tt, nr=nr: e.matmul(ps[0:NEXP, 0:nr], lhsT=gt[0:nr, tt, :], rhs=identf[0:nr, 0:nr], start=True, stop=True),
                         reads=["gwk", "identf"], writes=[pk])
                    S.op("dve", lambda e, ps=ps, r0=r0, nr=nr: e.tensor_copy(out=gatesT[:, r0:r0 + nr], in_=ps[0:NEXP, 0:nr]), reads=[pk], writes=["gatesT"])
                fence(["Ge"] + ["xnf%d" % t for t in range(5)])
                for ex in range(NEXP):
                    for t, (c0, w) in enumerate(TILES):
                        ps, pk = ps_gen()
                        S.op("pe", lambda e, ps=ps, ex=ex, c0=c0, w=w: e.matmul(ps[:, 0:w], lhsT=e8f[:, ex * 128:(ex + 1) * 128], rhs=gatesT[:, c0:c0 + w], start=True, stop=True),
                             reads=["e8f", "gatesT"], writes=[pk])
                        S.op("act", lambda e, ps=ps, c0=c0, w=w: e.copy(out=Ge[:, c0:c0 + w], in_=ps[:, 0:w]), reads=[pk], writes=["Ge"])
                    ffn_pass(mwg_d[ex], mwu_d[ex], mwd_d[ex], True)

            if stop_after == "ffn":
                S.emit(finals)
                return nc
            rmsnorm(l, V_GPLE)
            fence(R2KEYS)
            pTb = w3(R2, 2, NTOK, 2048)
            pv_ = pT_d[l].rearrange("(k p) n -> p k n", p=128)
            for (ca, cb_) in ((0, 1026), (1026, NTOK)):
                S.dma("pool", pTb[:, :, ca:cb_], pv_[:, :, ca:cb_], writes=["pTb"])
            sgp = r2f(0, 512)
            tp = r2f(1024, 512)
            for half in range(2):
                wpg, kpg = slab([(0, KC, 512, wview(wpg_d[l], half * 512, 512))])
                wpp, kpp = slab([(0, 2, 512, wview(wpp_d[l], half * 512, 512))])
                wpg3, wpp3 = w3(wpg, KC), w3(wpp, 2)
                for mm in range(4):
                    m = half * 4 + mm
                    for t, (c0, w) in enumerate(TILES):
                        psG, pkG = ps_gen()
                        proj(psG[:, 0:w], pkG, lambda k: wpg3[:, k, mm * 128:(mm + 1) * 128], KC, lambda k: xnT[:, k, c0:c0 + w], xnkeys(t), kpg)
                        psP, pkP = ps_gen()
                        proj(psP[:, 0:w], pkP, lambda k: wpp3[:, k, mm * 128:(mm + 1) * 128], 2, lambda k: pTb[:, k, c0:c0 + w], ["pTb", "pTb"], kpp)
                        S.op("act", lambda e, psG=psG, w=w: e.activation(out=sgp[:, 0:w], in_=psG[:, 0:w], func=AF.Sigmoid), reads=[pkG], writes=["sgp"])
                        S.op("dve", lambda e, psP=psP, w=w: e.tensor_tensor(out=tp[:, 0:w], in0=psP[:, 0:w], in1=sgp[:, 0:w], op=ALU.mult), reads=[pkP, "sgp"], writes=["tp"])
                        S.op("dve", lambda e, m=m, c0=c0, w=w: e.tensor_tensor(out=xT[:, m, c0:c0 + w], in0=tp[:, 0:w], in1=xT[:, m, c0:c0 + w], op=ALU.add),
                             reads=["tp", xk(t, m)], writes=[xk(t, m)])

        fi = [0]

        def fin(t, k, c0, w, g, rk):
            si = fi[0] % 2
            fi[0] += 1
            S.op("dve", lambda e: e.scalar_tensor_tensor(out=stg[si][:, 0:w], in0=xT[:, k, c0:c0 + w], scalar=g, in1=rstd[:, c0:c0 + w], op0=ALU.mult, op1=ALU.mult),
                 reads=[xk(t, k), rk, "vecs"], writes=["stg%d" % si])
            finals.append(S.dma("sp", yT_o[k * 128:(k + 1) * 128, c0:c0 + w], stg[si][:, 0:w], reads=["stg%d" % si]))

        rmsnorm(None, V_GFIN, fp32_out=fin)
        S.emit(finals)
    return nc


def _lg_ps(psb, t):
    return psb[4 + t] if t < 4 else psb[3]


def _lg_key(t):
    return "ps%d" % (4 + t) if t < 4 else "ps3"


def _pv(S, cur, PT, Vp, opad, pO, pS, nkj):
    pt_i, kj, hh, cs, cnt = cur
    first = (cnt == 0)
    lastg = (cnt == nkj - 1)
    S.op("pe", lambda e: e.matmul(pO[:, cs:512], lhsT=Vp[:, kj, hh, :], rhs=PT[pt_i][:, cs:512], start=first, stop=lastg),
         reads=["PT%d" % pt_i, "Vp"], writes=["ps6"])
    S.op("pe", lambda e: e.matmul(pS[:, cs:512], lhsT=opad[hh], rhs=PT[pt_i][:, cs:512], start=first, stop=lastg),
         reads=["PT%d" % pt_i, "cst"], writes=["ps7"])


def _constants():
    cst = np.zeros((128, 5 * 128), np.float32)
    cst[:, 0:128] = np.eye(128, dtype=np.float32)
    cst[:, 128:256] = 1.0
    kk, qq = np.meshgrid(np.arange(128), np.arange(128), indexing="ij")
    cst[:, 256:384] = np.where(kk > qq, -30000.0, 0.0)
    cst[:, 384:384 + 64] = 1.0
    cst[:, 512 + 64:512 + 128] = 1.0
    e8 = np.zeros((8, 8, 128), np.float32)
    for h in range(8):
        e8[h, h, :] = 1.0
    return cst, e8.reshape(8, 8 * 128)


def _onehot_rows():
    oh = np.zeros((NH, 8, T), np.float32)
    for h in range(NH):
        oh[h, h, :] = 1.0
    return oh


_NC_CACHE = {}
LAST_SCHED = None


def _decode_consts(b_f):
    cf = np.zeros((128, 256 + 2 * NH + 1), np.float32)
    ii, jj = np.meshgrid(np.arange(128), np.arange(128), indexing="ij")
    cf[:, 0:128] = (ii > jj).astype(np.float32)
    cf[:, 128:256] = 1.0
    cf[:, 256:256 + 2 * NH] = np.asarray(b_f, np.float32).reshape(1, 2 * NH)
    cf[:, 256 + 2 * NH] = np.arange(128, dtype=np.float32)
    return cf


def kernel(x_prompt, x_sample, p_prompt, p_sample, cache_k, cache_v, cache_logf, state_conv, state_h,
           page_table, g_mix, w_in, b_f, conv_w, conv_b, w_rg, b_rg, w_ig, b_ig, lru_lambda,
           w_a_up, w_b_up, w_out, g_ffn, dense_wg, dense_wu, dense_wd, moe_router, moe_wg, moe_wu,
           moe_wd, g_ple, w_ple_gate, w_ple_proj, g_final):
    f = lambda a: np.ascontiguousarray(np.asarray(a, dtype=np.float32))
    x_prompt, x_sample, p_prompt, p_sample = f(x_prompt), f(x_sample), f(p_prompt), f(p_sample)
    state_conv, state_h = f(state_conv), f(state_h)
    page_table = np.ascontiguousarray(np.asarray(page_table, dtype=np.int32))
    vecs = np.zeros((128, NV), np.float32)

    def put(base, v):
        vecs[:, base:base + 8] = f(v).reshape(8, 128).T

    for l in range(DEPTH):
        b = l * LV
        put(b + V_GMIX, g_mix[l])
        for j in range(4):
            put(b + V_CW + 8 * j, conv_w[l][j])
        put(b + V_CB, conv_b[l]); put(b + V_BRG, b_rg[l]); put(b + V_BIG, b_ig[l])
        put(b + V_LAM, lru_lambda[l]); put(b + V_GFFN, g_ffn[l]); put(b + V_GPLE, g_ple[l])
    put(V_GFIN, g_final)
    cst, e8 = _constants()
    shared = dict(
        vecs=vecs, bfT=f(f(b_f).T), w_in=f(w_in), w_rg=f(w_rg), w_ig=f(w_ig), w_a_up=f(w_a_up), w_b_up=f(w_b_up),
        w_out=f(w_out), dense_wg=f(dense_wg)[0], dense_wu=f(dense_wu)[0], dense_wd=f(dense_wd)[0],
        router=f(moe_router)[0], moe_wg=f(moe_wg)[0], moe_wu=f(moe_wu)[0], moe_wd=f(moe_wd)[0],
        w_ple_gate=f(w_ple_gate), w_ple_proj=f(w_ple_proj), cst=cst, e8=e8, oh=_onehot_rows(),
        ck=f(cache_k).reshape(DEPTH, NPOOL, 128, 512), cv=f(cache_v).reshape(DEPTH, NPOOL, 128, 512),
        clf=f(cache_logf), cf=_decode_consts(b_f))
    in_maps = []
    for c in range(NCORES):
        sl = slice(c * NS, (c + 1) * NS)
        m = dict(shared)
        m["xT"] = f(np.concatenate([x_prompt[c].T, x_sample[sl, 0, :].T], axis=1))
        m["pT"] = f(np.stack([np.concatenate([p_prompt[l, c].T, p_sample[l, sl, 0, :].T], axis=1) for l in range(DEPTH)]))
        m["scT"] = f(np.transpose(state_conv[:, sl], (0, 3, 2, 1)))
        m["shT"] = f(np.transpose(state_h[:, sl], (0, 2, 1)))
        m["pt"] = np.ascontiguousarray(page_table[sl].reshape(1, NS * NPG))
        in_maps.append(m)
    if "nc" not in _NC_CACHE:
        _NC_CACHE["nc"] = build_program()
    res = run_bass_kernel_spmd(_NC_CACHE["nc"], in_maps, core_ids=list(range(NCORES)))
    return assemble(res.results)


def assemble(R):
    B = len(R)
    cat = lambda xs: np.concatenate(xs, axis=0)
    y_prompt = np.stack([R[c]["yT"][:, :T].T for c in range(B)])
    y_sample = cat([R[c]["yT"][:, T:].T for c in range(B)])[:, None, :]
    kp = np.stack([np.stack([R[c]["kT"][l][:, :T].T for c in range(B)]) for l in range(DEPTH)]).reshape(DEPTH, B, T, NH, DH)
    vp = np.stack([np.stack([R[c]["v"][l][:T] for c in range(B)]) for l in range(DEPTH)]).reshape(DEPTH, B, T, NH, DH)
    lp = np.stack([np.stack([R[c]["lfT"][l][:, :T].T for c in range(B)]) for l in range(DEPTH)])
    cp = np.stack([np.stack([R[c]["cvT"][l].T for c in range(B)]) for l in range(DEPTH)])
    hp = np.stack([np.stack([R[c]["hT"][l][:, 0] for c in range(B)]) for l in range(DEPTH)])
    ks = np.stack([cat([R[c]["kT"][l][:, T:].T for c in range(B)]) for l in range(DEPTH)]).reshape(DEPTH, B * NS, 1, NH, DH)
    vs = np.stack([cat([R[c]["v"][l][T:] for c in range(B)]) for l in range(DEPTH)]).reshape(DEPTH, B * NS, 1, NH, DH)
    ls = np.stack([cat([R[c]["lfT"][l][:, T:].T for c in range(B)]) for l in range(DEPTH)]).reshape(DEPTH, B * NS, 1, NH)
    cs = np.stack([cat([np.transpose(R[c]["cvS"][l], (2, 1, 0)) for c in range(B)]) for l in range(DEPTH)])
    hs = np.stack([cat([R[c]["hT"][l][:, 1:].T for c in range(B)]) for l in range(DEPTH)])
    c32 = lambda a: np.ascontiguousarray(a, dtype=np.float32)
    return tuple(c32(a) for a in (y_prompt, y_sample, kp, vp, lp, cp, hp, ks, vs, ls, cs, hs))
```

```python
import contextlib
import types
import numpy as np
import concourse.bass as bass
import concourse.mybir as mybir
from concourse.bass_utils import run_bass_kernel_spmd

F32 = mybir.dt.float32
BF16 = mybir.dt.bfloat16
I32 = mybir.dt.int32
AF = mybir.ActivationFunctionType
ALU = mybir.AluOpType
AX = mybir.AxisListType

NCORES = 8
D = 1024
KC = 8
T = 2048
NS = 4
NTOK = T + NS
TILES = [(0, 512), (512, 512), (1024, 512), (1536, 512), (2048, NS)]
NH = 8
DH = 64
DFF = 3584
FCH = DFF // 128
NEXP = 8
DPLE = 256
NIN = 5640
EPS = 1e-6
NPG = 64
NPOOL = 2560
DEPTH = 2
OQ, OK_, OV, OFL, OXR, OGR, OGA, OGB = 0, 512, 1024, 1536, 1544, 2568, 3592, 4616
V_GMIX, V_CW, V_CB, V_BRG, V_BIG, V_LAM, V_GFFN, V_GPLE, LV = 0, 8, 40, 48, 56, 64, 72, 80, 88
V_GFIN = DEPTH * LV
NV = V_GFIN + 8


def _freeze(fn):
    if fn.__closure__ is None:
        return fn
    cells = []
    for c in fn.__closure__:
        try:
            cells.append(types.CellType(c.cell_contents))
        except ValueError:
            cells.append(c)
    return types.FunctionType(fn.__code__, fn.__globals__, fn.__name__, fn.__defaults__, tuple(cells))


class Op:
    __slots__ = ("eng", "fn", "deps", "signaled", "seq", "is_dma", "sem", "val", "idx")

    def __init__(self, eng, fn, deps, is_dma):
        self.eng, self.fn, self.deps, self.is_dma = eng, fn, deps, is_dma
        self.signaled, self.seq, self.sem, self.val, self.idx = False, 0, None, 0, 0


class Sched:
    ENGS = ("pe", "act", "dve", "pool", "sp")
    NDMASEM = 12

    def __init__(self, nc):
        self.nc = nc
        self.ops = {e: [] for e in self.ENGS}
        self.last_w, self.readers = {}, {}
        self.dma_count = {e: 0 for e in self.ENGS}
        self.dma_ops = {e: [] for e in self.ENGS}

    def _hazards(self, reads, writes):
        deps = []
        for k in reads:
            w = self.last_w.get(k)
            if w is not None:
                deps.append(w)
        for k in writes:
            w = self.last_w.get(k)
            if w is not None:
                deps.append(w)
            deps.extend(self.readers.get(k, ()))
        return deps

    def _commit(self, op, reads, writes):
        for k in reads:
            self.readers.setdefault(k, []).append(op)
        for k in writes:
            self.last_w[k] = op
            self.readers[k] = []

    def op(self, eng, fn, reads=(), writes=(), deps=()):
        o = Op(eng, _freeze(fn), list(deps) + self._hazards(reads, writes), False)
        self.ops[eng].append(o)
        self._commit(o, reads, writes)
        return o

    def dma(self, eng, out, in_, reads=(), writes=(), deps=(), **kw):
        d = list(deps) + self._hazards(reads, writes)
        i = self.dma_count[eng]
        self.dma_count[eng] += 1
        if i >= self.NDMASEM:
            d.append(self.dma_ops[eng][i - self.NDMASEM])
        o = Op(eng, lambda e: e.dma_start(out=out, in_=in_, **kw), d, True)
        o.idx = i
        self.dma_ops[eng].append(o)
        self.ops[eng].append(o)
        self._commit(o, reads, writes)
        return o

    def dma_fn(self, eng, fn, reads=(), writes=(), deps=()):
        d = list(deps) + self._hazards(reads, writes)
        i = self.dma_count[eng]
        self.dma_count[eng] += 1
        if i >= self.NDMASEM:
            d.append(self.dma_ops[eng][i - self.NDMASEM])
        o = Op(eng, _freeze(fn), d, True)
        o.idx = i
        self.dma_ops[eng].append(o)
        self.ops[eng].append(o)
        self._commit(o, reads, writes)
        return o

    def emit(self, final_ops):
        nc = self.nc
        for e in self.ENGS:
            for o in self.ops[e]:
                for d in o.deps:
                    if d.is_dma or (d.eng == "pe" and o.eng == "pe" and not o.is_dma):
                        continue
                    d.signaled = True
        for o in final_ops:
            if not o.is_dma:
                o.signaled = True
        for e in self.ENGS:
            n = 0
            for o in self.ops[e]:
                if not o.is_dma and o.signaled:
                    n += 1
                    o.seq = n
        with contextlib.ExitStack() as st:
            esem = {e: st.enter_context(nc.semaphore("s_" + e)) for e in self.ENGS}
            dsem = {e: [st.enter_context(nc.semaphore("d_%s%d" % (e, i))) for i in range(self.NDMASEM)]
                    for e in self.ENGS if self.dma_count[e] > 0}
            for e in self.ENGS:
                for o in self.ops[e]:
                    if o.is_dma:
                        o.sem, o.val = dsem[e][o.idx % self.NDMASEM], 16 * (o.idx // self.NDMASEM + 1)
                    else:
                        o.sem, o.val = esem[e], o.seq
            block = st.enter_context(nc.Block())

            def run(engname, engobj):
                waited = {}
                for o in self.ops[engname]:
                    need = {}
                    for d in o.deps:
                        if (not d.is_dma) and d.eng == "pe" and engname == "pe" and not o.is_dma:
                            continue
                        key = id(d.sem)
                        if waited.get(key, 0) >= d.val:
                            continue
                        if key not in need or need[key][1] < d.val:
                            need[key] = (d.sem, d.val)
                    for key, (s, v) in need.items():
                        engobj.wait_ge(s, v)
                        waited[key] = v
                    inst = o.fn(engobj)
                    if o.is_dma:
                        inst.then_inc(o.sem, 16)
                    elif o.signaled:
                        inst.then_inc(o.sem, 1)
                if engname == "sp":
                    for o in final_ops:
                        key = id(o.sem)
                        if waited.get(key, 0) < o.val:
                            engobj.wait_ge(o.sem, o.val)
                            waited[key] = o.val

            block.tensor(lambda eng: run("pe", eng))
            block.scalar(lambda eng: run("act", eng))
            block.vector(lambda eng: run("dve", eng))
            block.gpsimd(lambda eng: run("pool", eng))
            block.sync(lambda eng: run("sp", eng))


def build_program(depth=DEPTH, stop_after=None):
    nc = bass.Bass("TRN2", target_bir_lowering=False)
    S = Sched(nc)
    global LAST_SCHED
    LAST_SCHED = S

    def din(name, shape, dt=F32):
        return nc.dram_tensor(name, list(shape), dt, kind="ExternalInput").ap()

    def dout(name, shape, dt=F32):
        return nc.dram_tensor(name, list(shape), dt, kind="ExternalOutput").ap()

    xT_d = din("xT", [D, NTOK])
    pT_d = din("pT", [DEPTH, DPLE, NTOK])
    scT_d = din("scT", [DEPTH, D, 3, NS])
    shT_d = din("shT", [DEPTH, D, NS])
    vecs_d = din("vecs", [128, NV])
    bfT_d = din("bfT", [NH, DEPTH])
    w_in_d = din("w_in", [DEPTH, D, NIN])
    w_rg_d = din("w_rg", [DEPTH, KC, 128, 128])
    w_ig_d = din("w_ig", [DEPTH, KC, 128, 128])
    w_a_d = din("w_a_up", [DEPTH, 512, D])
    w_b_d = din("w_b_up", [DEPTH, D, D])
    w_o_d = din("w_out", [DEPTH, D, D])
    dwg_d = din("dense_wg", [D, DFF])
    dwu_d = din("dense_wu", [D, DFF])
    dwd_d = din("dense_wd", [DFF, D])
    rt_d = din("router", [D, NEXP])
    mwg_d = din("moe_wg", [NEXP, D, DFF])
    mwu_d = din("moe_wu", [NEXP, D, DFF])
    mwd_d = din("moe_wd", [NEXP, DFF, D])
    wpg_d = din("w_ple_gate", [DEPTH, D, D])
    wpp_d = din("w_ple_proj", [DEPTH, DPLE, D])
    cst_d = din("cst", [128, 5 * 128])
    e8_d = din("e8", [8, 8 * 128])
    oh_d = din("oh", [NH, 8, T])
    ck_d = din("ck", [DEPTH, NPOOL, 128, 512])
    cv_d = din("cv", [DEPTH, NPOOL, 128, 512])
    clf_d = din("clf", [DEPTH, NPOOL, 128, NH])
    pt_d = din("pt", [1, NS * NPG], I32)
    cf_d = din("cf", [128, 256 + 2 * NH + 1])

    yT_o = dout("yT", [D, NTOK])
    kT_o = dout("kT", [DEPTH, 512, NTOK])
    v_o = dout("v", [DEPTH, NTOK, 512])
    lfT_o = dout("lfT", [DEPTH, NH, NTOK])
    cvT_o = dout("cvT", [DEPTH, D, 3])
    cvS_o = dout("cvS", [DEPTH, D, 3, NS])
    hT_o = dout("hT", [DEPTH, D, 1 + NS])
    finals = []

    with contextlib.ExitStack() as st:
        def sb(name, shape, dt):
            return st.enter_context(nc.sbuf_tensor(name, list(shape), dt))

        xT = sb("xT_s", [128, KC, NTOK], F32)
        xnT = sb("xnT", [128, KC, NTOK], BF16)
        R1 = sb("R1", [128, 4 * NTOK], BF16)
        R2 = sb("R2", [128, 18432], BF16)
        ring = [sb("ring%d" % i, [128, 4096], BF16) for i in range(3)]
        vecs = sb("vecs_s", [128, NV], F32)
        lsc = sb("lsc", [128, DEPTH * KC], F32)
        cst = sb("cst_s", [128, 5 * 128], BF16)
        identf = sb("identf", [128, 128], F32)
        cf = sb("cf_s", [128, 256 + 2 * NH + 1], F32)
        idx = sb("idx", [128, NS * NPG], I32)
        bfn = sb("bfn", [NH, DEPTH], F32)
        rstd = sb("rstd", [128, NTOK], F32)
        sqt = sb("sqt", [128, 1, 512], BF16)
        stg = [sb("stg%d" % i, [128, 512], F32) for i in range(2)]
        wgate = sb("wgate", [128, 2, KC, 128], BF16)
        carry = sb("carry", [128, KC, 3], F32)
        hcar = sb("hcar", [128, KC], F32)
        prevS = sb("prevS", [128, KC, 3, NS], F32)
        h0S = sb("h0S", [128, KC, NS], F32)
        xrS = sb("xrS", [128, KC, NS], F32)
        hS = sb("hS_s", [128, KC, 1 + NS], F32)
        c8b = sb("c8b", [NH, T], BF16)
        negc = sb("negc", [128, 16, NH], F32)
        rtw = sb("rtw", [128, KC, NEXP], F32)
        fsc = sb("fsc", [128, 4], F32)
        psb = [st.enter_context(nc.psum_tensor("ps%d" % i, [128, 512], F32)) for i in range(8)]

        ident = cst[:, 0:128]
        onesb = cst[:, 128:256]
        mneg = cst[:, 256:384]
        opad = [cst[:, 384:512], cst[:, 512:640]]
        utri = cf[:, 0:128]
        onesf = cf[:, 128:256]
        bfrow = cf[:, 256:256 + 2 * NH]

        def r2f(off, n):
            return R2[:, off:off + 2 * n].bitcast(F32)

        R2KEYS = (["y%d" % k for k in range(KC)] + ["t1", "t2", "t3", "xcb", "sgt", "qT", "kTb", "qTs", "kTs", "Vp", "attnS"]
                  + ["attn%d" % k for k in range(4)] + ["PT%d" % k for k in range(3)]
                  + ["sga", "tmpm", "sgf", "tf", "lgS", "gatesT", "Ge", "e8f", "gwk", "lgt", "m12", "sgp", "tp", "pTb",
                     "lfT", "cT", "ones8"] + ["xnf%d" % t for t in range(5)])
        RSKEYS = ["rstd%d" % t for t in range(5)]
        DKEYS = ["ptb", "ptf", "pg0", "pg1", "pg2", "pg3", "pg4", "pg5", "dX", "dSP", "dlat", "dqs", "dvwn", "dmisc"]
        R1KEYS = DKEYS + ["y%d" % k for k in range(KC)] + ["mg%d" % m for m in range(KC)] + ["h%d_%d" % (f_, t) for f_ in range(4) for t in range(5)]

        def fence(keys):
            S.op("dve", lambda e: e.memset(fsc[:], 0.0), writes=list(dict.fromkeys(keys)) + ["fsc"])

        psi = [0]
        psrot = [list(range(8))]

        def ps_gen():
            rot = psrot[0]
            i = rot[psi[0] % len(rot)]
            psi[0] += 1
            return psb[i], "ps%d" % i

        ringi = [0]

        def slab(parts):
            i = ringi[0] % len(ring)
            ringi[0] += 1
            key = "ring%d" % i
            for (off, k, n, src) in parts:
                dst = ring[i][:, off:off + k * n].rearrange("p (k n) -> p k n", k=k)
                S.dma("pool", dst, src, writes=[key])
            return ring[i], key

        def wview(w2d, c0, n):
            return w2d.rearrange("(k p) n -> p k n", p=128)[:, :, c0:c0 + n]

        def w3(buf, k, n=512, off=0):
            return buf[:, off:off + k * n].rearrange("p (k n) -> p k n", k=k)

        def xk(t, k):
            return "x%d_%d" % (k, t)

        def xnk(t, k):
            return "xn%d_%d" % (k, t)

        def xnkeys(t):
            return [xnk(t, k) for k in range(KC)]

        def proj(out_ps, pskey, lhs_fn, nk, rhs_fn, rkeys, wkey):
            for k in range(nk):
                L_, R_ = lhs_fn(k), rhs_fn(k)
                S.op("pe", (lambda e, k=k, L_=L_, R_=R_: e.matmul(out_ps, lhsT=L_, rhs=R_, start=(k == 0), stop=(k == nk - 1))),
                     reads=[wkey, rkeys[k]], writes=[pskey])

        S.dma("sp", xT[:], xT_d.rearrange("(k p) n -> p k n", p=128), writes=[xk(t, k) for t in range(5) for k in range(KC)])
        S.dma("sp", vecs[:], vecs_d, writes=["vecs"])
        S.dma("pool", cst[:], cst_d, writes=["cst"])
        S.dma("sp", identf[:], cst_d[:, 0:128], writes=["identf"])
        S.dma("sp", cf[:], cf_d, writes=["cf"])
        ptb = R1[:, 0:2 * NS * NPG].bitcast(I32)
        ptf = R1[:, 2 * NS * NPG:4 * NS * NPG].bitcast(F32)
        S.dma("sp", ptb, pt_d.partition_broadcast(128), writes=["ptb"])
        S.op("dve", lambda e: e.tensor_copy(out=ptf, in_=ptb), reads=["ptb"], writes=["ptf"])
        S.op("dve", lambda e: e.tensor_scalar(out=ptf, in0=ptf, scalar1=128.0, scalar2=cf[:, 256 + 2 * NH:256 + 2 * NH + 1], op0=ALU.mult, op1=ALU.add),
             reads=["ptf", "cf"], writes=["ptf"])
        S.op("dve", lambda e: e.tensor_copy(out=idx[:], in_=ptf), reads=["ptf"], writes=["idx"])
        S.dma("sp", bfn[:], bfT_d, writes=["bfn"])
        S.op("dve", lambda e: e.tensor_scalar(out=bfn[:], in0=bfn[:], scalar1=-1.0, scalar2=None, op0=ALU.mult), reads=["bfn"], writes=["bfn"])
        for l in range(depth):
            src = vecs[:, l * LV + V_LAM:l * LV + V_LAM + KC]
            dst = lsc[:, l * KC:(l + 1) * KC]
            S.op("act", lambda e, s=src, d=dst: e.activation(out=d, in_=s, func=AF.Exp, scale=-1.0), reads=["vecs"], writes=["lsc"])
            S.op("act", lambda e, d=dst: e.activation(out=d, in_=d, func=AF.Ln, bias=1.0), reads=["lsc"], writes=["lsc"])
            S.op("dve", lambda e, d=dst: e.tensor_scalar(out=d, in0=d, scalar1=-8.0, scalar2=None, op0=ALU.mult), reads=["lsc"], writes=["lsc"])

        def vcol(l, base, k):
            c = (l * LV if l is not None else 0) + base + k
            return vecs[:, c:c + 1]

        def rmsnorm(gl, gbase, fp32_out=None):
            for t, (c0, w) in enumerate(TILES):
                ps, pk = ps_gen()
                for k in range(KC):
                    s_ = sqt[:, 0, 0:w]
                    sk = "sqt0"
                    S.op("act", lambda e, s_=s_, k=k, c0=c0, w=w: e.activation(out=s_, in_=xT[:, k, c0:c0 + w], func=AF.Square), reads=[xk(t, k)], writes=[sk])
                    S.op("pe", lambda e, s_=s_, k=k, ps=ps, w=w: e.matmul(ps[:, 0:w], lhsT=onesb, rhs=s_, start=(k == 0), stop=(k == KC - 1)),
                         reads=[sk, "cst"], writes=[pk])
                rk = "rstd%d" % t
                S.op("act", lambda e, ps=ps, c0=c0, w=w: e.activation(out=rstd[:, c0:c0 + w], in_=ps[:, 0:w], func=AF.Sqrt, scale=1.0 / D, bias=EPS), reads=[pk], writes=[rk])
                S.op("dve", lambda e, c0=c0, w=w: e.reciprocal(out=rstd[:, c0:c0 + w], in_=rstd[:, c0:c0 + w]), reads=[rk], writes=[rk])
                for k in range(KC):
                    g = vcol(gl, gbase, k)
                    if fp32_out is None:
                        S.op("dve", lambda e, k=k, c0=c0, w=w, g=g: e.scalar_tensor_tensor(
                            out=xnT[:, k, c0:c0 + w], in0=xT[:, k, c0:c0 + w], scalar=g, in1=rstd[:, c0:c0 + w], op0=ALU.mult, op1=ALU.mult),
                            reads=[xk(t, k), rk, "vecs"], writes=[xnk(t, k)])
                    else:
                        fp32_out(t, k, c0, w, g, rk)

        def evac_add_x(ps, pk, t, m, c0, w):
            S.op("dve", lambda e: e.tensor_tensor(out=xT[:, m, c0:c0 + w], in0=ps[:, 0:w], in1=xT[:, m, c0:c0 + w], op=ALU.add),
                 reads=[pk, xk(t, m)], writes=[xk(t, m)])

        def decode_gen(l, attnT):
            w_in_l = w_in_d[l]
            f32v = lambda off, n: R1[:, off:off + 2 * n].bitcast(F32)
            rsb = rstd[:, :].bitcast(BF16)
            pg = [f32v(i * 1024, 512) for i in range(3)] + [rsb[:, i * 1024:(i + 1) * 1024].bitcast(F32) for i in range(3)]
            dX = f32v(3072, 512)
            dXb = [R1[:, 3072:3584], R1[:, 3584:4096]]
            dSP = f32v(4096, 512)
            dlat = f32v(5120, 520)
            dqs = f32v(6160, 512)
            dvwn = R1[:, 7184:7696]
            dm = f32v(7696, 64)
            lfs, pnew, snew = dm[0:NS, 0:8], dm[0:NS, 8:16], dm[0:NS, 16:24]
            lfnb, rs, rden = dm[:, 24:32], dm[:, 32:40], dm[0:1, 40:48]
            SP3 = dSP.rearrange("p (j h) -> p j h", h=NH)
            X3 = dX.rearrange("p (j h) -> p j h", h=NH)
            lat3 = dlat[:, 0:512].rearrange("p (j h) -> p j h", h=NH)
            xs_keys = xnkeys(4)
            dsts = [dqs[0:NS, :], pg[0][0:NS, :], pg[1][0:NS, :]]
            dkeys = ["dqs", "pg0", "pg1"]
            for i, c0 in enumerate((OQ, OK_, OV)):
                wsl, kw = slab([(0, KC, 512, wview(w_in_l, c0, 512))])
                wsl3 = w3(wsl, KC)
                ps, pk = ps_gen()
                proj(ps[0:NS, :], pk, lambda k: xnT[:, k, T:T + NS], KC, lambda k: wsl3[:, k, :], xs_keys, kw)
                S.op("dve", lambda e, ps=ps, i=i: e.tensor_copy(out=dsts[i], in_=ps[0:NS, :]), reads=[pk], writes=[dkeys[i]])
            wsl, kw = slab([(0, KC, 8, wview(w_in_l, OFL, 8))])
            wsl3 = w3(wsl, KC, 8)
            ps, pk = ps_gen()
            proj(ps[0:NS, 0:NH], pk, lambda k: xnT[:, k, T:T + NS], KC, lambda k: wsl3[:, k, :], xs_keys, kw)
            S.op("dve", lambda e, ps=ps: e.tensor_tensor(out=lfs, in0=ps[0:NS, 0:NH], in1=bfrow[0:NS, l * NH:(l + 1) * NH], op=ALU.add), reads=[pk, "cf"], writes=["dmisc"])
            S.op("act", lambda e: e.activation(out=lfs, in_=lfs, func=AF.Exp, scale=-1.0), reads=["dmisc"], writes=["dmisc"])
            S.op("act", lambda e: e.activation(out=lfs, in_=lfs, func=AF.Ln, bias=1.0), reads=["dmisc"], writes=["dmisc"])
            S.op("dve", lambda e: e.tensor_scalar(out=lfs, in0=lfs, scalar1=-1.0, scalar2=None, op0=ALU.mult), reads=["dmisc"], writes=["dmisc"])
            S.op("dve", lambda e: e.tensor_tensor(out=pg[2][0:NS, :], in0=dqs[0:NS, :], in1=pg[0][0:NS, :], op=ALU.mult), reads=["dqs", "pg0"], writes=["pg2"])
            S.op("dve", lambda e: e.tensor_reduce(out=snew, in_=pg[2][0:NS, :].rearrange("p (h d) -> p h d", h=NH), axis=AX.X, op=ALU.add), reads=["pg2"], writes=["dmisc"])
            S.op("act", lambda e: e.activation(out=pnew, in_=snew, func=AF.Exp, scale=0.125), reads=["dmisc"], writes=["dmisc"])
            S.op("dve", lambda e: e.tensor_tensor(out=dvwn[0:NS, :].rearrange("p (h d) -> p h d", h=NH), in0=pg[1][0:NS, :].rearrange("p (h d) -> p h d", h=NH),
                                                  in1=pnew.unsqueeze(2).broadcast_to([NS, NH, DH]), op=ALU.mult), reads=["pg1", "dmisc"], writes=["dvwn"])
            yield

            def page_dma(dram4, b, j, dst, key, deps=()):
                src2d = dram4.rearrange("l n p f -> (l n p) f")
                col = b * NPG + j
                eoff = l * NPOOL * 128 * int(dram4.shape[3])

                def fn(e):
                    return e.indirect_dma_start(out=dst, out_offset=None, in_=src2d,
                                                in_offset=bass.IndirectOffsetOnAxis(ap=idx[:, col:col + 1], axis=0),
                                                element_offset=eoff)
                return S.dma_fn("pool", fn, reads=["idx"], writes=[key], deps=deps)

            for b in range(NS):
                S.op("dve", lambda e: e.tensor_scalar(out=pg[2][0:NS, :], in0=dqs[0:NS, :], scalar1=identf[0:NS, b:b + 1], scalar2=None, op0=ALU.mult),
                     reads=["dqs", "identf"], writes=["pg2"])
                ps, pk = ps_gen()
                S.op("pe", lambda e, ps=ps: e.matmul(ps[:, :], lhsT=onesf[0:NS, :], rhs=pg[2][0:NS, :], start=True, stop=True), reads=["pg2", "cf"], writes=[pk])
                S.op("act", lambda e, ps=ps: e.activation(out=dlat[:, 0:512], in_=ps[:, :], func=AF.Identity, scale=0.125), reads=[pk], writes=["dlat"])
                S.op("dve", lambda e: e.tensor_scalar(out=dm[0:NS, 48:56], in0=lfs, scalar1=identf[0:NS, b:b + 1], scalar2=None, op0=ALU.mult), reads=["dmisc", "identf"], writes=["dmisc"])
                ps, pk = ps_gen()
                S.op("pe", lambda e, ps=ps: e.matmul(ps[:, 0:NH], lhsT=onesf[0:NS, :], rhs=dm[0:NS, 48:56], start=True, stop=True), reads=["dmisc", "cf"], writes=[pk])
                S.op("dve", lambda e, ps=ps: e.tensor_copy(out=lfnb, in_=ps[:, 0:NH]), reads=[pk], writes=["dmisc"])
                yield
                for j in range(NPG):
                    pi = j % 6
                    page_dma(ck_d, b, j, pg[pi], "pg%d" % pi)
                    S.op("dve", lambda e, pi=pi: e.tensor_tensor(out=dX, in0=pg[pi], in1=dlat[:, 0:512], op=ALU.mult), reads=["pg%d" % pi, "dlat"], writes=["dX"])
                    lastred = S.op("dve", lambda e, j=j: e.tensor_reduce(out=SP3[:, j, :], in_=dX.rearrange("p (h d) -> p h d", h=NH), axis=AX.X, op=ALU.add), reads=["dX"], writes=["dSP"])
                    yield
                lkeys = ["dXl%d" % j for j in range(NPG)]
                for j in range(NPG):
                    page_dma(clf_d, b, j, X3[:, j, :], lkeys[j], deps=[lastred])
                psA, pkA = ps_gen()
                S.op("pe", lambda e, psA=psA: e.matmul(psA[:, :], lhsT=utri, rhs=dX, start=True, stop=True), reads=["dX", "cf"] + lkeys, writes=[pkA])
                psB, pkB = ps_gen()
                S.op("pe", lambda e, psB=psB: e.matmul(psB[:, :], lhsT=onesf, rhs=dX, start=True, stop=True), reads=["dX", "cf"] + lkeys, writes=[pkB])
                S.op("dve", lambda e, psA=psA: e.tensor_tensor(out=dSP, in0=psA[:, :], in1=dSP, op=ALU.add), reads=[pkA, "dSP"], writes=["dSP"])
                S.op("dve", lambda e, psB=psB: e.tensor_copy(out=dlat[:, 0:512], in_=psB[:, :]), reads=[pkB], writes=["dlat"])
                for h in range(NH):
                    init = 0.0
                    S.op("dve", lambda e, h=h: e.tensor_tensor_scan(out=X3[:, :, h], data0=onesf[:, 0:NPG], data1=lat3[:, :, h], initial=0.0, op0=ALU.mult, op1=ALU.add),
                         reads=["dlat", "cf"], writes=["dX"])
                S.op("dve", lambda e: e.tensor_tensor(out=lat3, in0=X3[:, NPG - 1:NPG, :].broadcast_to([128, NPG, NH]), in1=X3, op=ALU.subtract), reads=["dX"], writes=["dlat"])
                S.op("dve", lambda e: e.tensor_tensor(out=dSP, in0=dSP, in1=dlat[:, 0:512], op=ALU.add), reads=["dSP", "dlat"], writes=["dSP"])
                S.op("dve", lambda e: e.tensor_tensor(out=SP3, in0=SP3, in1=lfnb.unsqueeze(1).broadcast_to([128, NPG, NH]), op=ALU.add), reads=["dSP", "dmisc"], writes=["dSP"])
                S.op("act", lambda e: e.activation(out=dSP, in_=dSP, func=AF.Exp), reads=["dSP"], writes=["dSP"])
                S.op("dve", lambda e: e.tensor_reduce(out=rs, in_=dSP.rearrange("p (j h) -> p h j", h=NH), axis=AX.X, op=ALU.add), reads=["dSP"], writes=["dmisc"])
                yield
                acc, ak = psb[3], "ps3"
                for j in range(NPG):
                    pi = j % 6
                    page_dma(cv_d, b, j, pg[pi], "pg%d" % pi)
                    vw = dXb[j % 2]
                    S.op("dve", lambda e, pi=pi, vw=vw, j=j: e.tensor_tensor(out=vw.rearrange("p (h d) -> p h d", h=NH), in0=pg[pi].rearrange("p (h d) -> p h d", h=NH),
                                                                           in1=SP3[:, j, :].unsqueeze(2).broadcast_to([128, NH, DH]), op=ALU.mult),
                         reads=["pg%d" % pi, "dSP"], writes=["dXb%d" % (j % 2), "dX"])
                    S.op("pe", lambda e, vw=vw, j=j: e.matmul(acc[0:1, :], lhsT=onesb[:, 0:1], rhs=vw, start=(j == 0), stop=False), reads=["dXb%d" % (j % 2), "cst"], writes=[ak])
                    yield
                S.op("pe", lambda e: e.matmul(acc[0:1, :], lhsT=ident[0:NS, b:b + 1], rhs=dvwn[0:NS, :], start=False, stop=True), reads=["dvwn", "cst"], writes=[ak])
                psD, pkD = ps_gen()
                S.op("pe", lambda e, psD=psD: e.matmul(psD[0:1, 0:NH], lhsT=onesf[:, 0:1], rhs=rs, start=True, stop=False), reads=["dmisc", "cf"], writes=[pkD])
                S.op("pe", lambda e, psD=psD: e.matmul(psD[0:1, 0:NH], lhsT=identf[0:NS, b:b + 1], rhs=pnew, start=False, stop=True), reads=["dmisc", "identf"], writes=[pkD])
                S.op("dve", lambda e, psD=psD: e.reciprocal(out=rden, in_=psD[0:1, 0:NH]), reads=[pkD], writes=["dmisc"])
                orow = pg[0][0:1, :]
                S.op("dve", lambda e: e.tensor_tensor(out=orow.rearrange("p (h d) -> p h d", h=NH), in0=acc[0:1, :].rearrange("p (h d) -> p h d", h=NH),
                                                      in1=rden.unsqueeze(2).broadcast_to([1, NH, DH]), op=ALU.mult), reads=[ak, "dmisc"], writes=["pg0"])
                psC, pkC = ps_gen()
                for c in range(4):
                    S.op("pe", lambda e, psC=psC, c=c: e.matmul(psC[:, c:c + 1], lhsT=orow[:, c * 128:(c + 1) * 128], rhs=identf[0:1, 0:1], start=True, stop=True),
                         reads=["pg0", "identf"], writes=[pkC])
                S.op("dve", lambda e, psC=psC: e.tensor_copy(out=attnT[:, :, T + b], in_=psC[:, 0:4]), reads=[pkC], writes=["attnS"])
                yield

        for l in range(depth):
            w_in_l = w_in_d[l]
            S.dma("pool", wgate[:, 0], w_rg_d[l].rearrange("n c d -> c n d"), writes=["wgate"])
            S.dma("pool", wgate[:, 1], w_ig_d[l].rearrange("n c d -> c n d"), writes=["wgate"])
            rmsnorm(l, V_GMIX)
            fence(R2KEYS + R1KEYS)

            lfT = r2f(8512, NTOK)[0:NH]
            cT = r2f(12672, T)[0:NH]
            ones8 = r2f(16768, 512)[0:NH]
            S.op("dve", lambda e: e.memset(ones8, 1.0), writes=["ones8"])
            wfl, kfl = slab([(0, KC, 8, wview(w_in_l, OFL, 8))])
            wfl3 = w3(wfl, KC, 8)
            for t, (c0, w) in enumerate(TILES):
                ps, pk = ps_gen()
                proj(ps[0:NH, 0:w], pk, lambda k: wfl3[:, k, :], KC, lambda k: xnT[:, k, c0:c0 + w], xnkeys(t), kfl)
                S.op("act", lambda e, ps=ps, c0=c0, w=w: e.activation(out=lfT[:, c0:c0 + w], in_=ps[0:NH, 0:w], func=AF.Exp, scale=-1.0, bias=bfn[:, l:l + 1]),
                     reads=[pk, "bfn"], writes=["lfT"])
                S.op("act", lambda e, c0=c0, w=w: e.activation(out=lfT[:, c0:c0 + w], in_=lfT[:, c0:c0 + w], func=AF.Ln, bias=1.0), reads=["lfT"], writes=["lfT"])
                S.op("dve", lambda e, c0=c0, w=w: e.tensor_scalar(out=lfT[:, c0:c0 + w], in0=lfT[:, c0:c0 + w], scalar1=-1.0, scalar2=None, op0=ALU.mult),
                     reads=["lfT"], writes=["lfT"])
                if t < 4:
                    init = 0.0 if t == 0 else cT[:, c0 - 1:c0]
                    S.op("dve", lambda e, c0=c0, w=w, init=init: e.tensor_tensor_scan(out=cT[:, c0:c0 + w], data0=ones8[:, 0:w], data1=lfT[:, c0:c0 + w],
                                                                                     initial=init, op0=ALU.mult, op1=ALU.add),
                         reads=["lfT", "ones8", "cT"], writes=["cT"])
            finals.append(S.dma("sp", lfT_o[l], lfT, reads=["lfT"]))
            S.op("act", lambda e: e.activation(out=c8b[:], in_=cT, func=AF.Identity, scale=8.0), reads=["cT"], writes=["c8b"])
            for g4 in range(4):
                ps, pk = ps_gen()
                for j in range(4):
                    tt = g4 * 4 + j
                    S.op("pe", lambda e, ps=ps, j=j, tt=tt: e.matmul(ps[:, j * 8:(j + 1) * 8], lhsT=cT[:, tt * 128:(tt + 1) * 128], rhs=identf[0:NH, 0:NH], start=True, stop=True),
                         reads=["cT", "identf"], writes=[pk])
                S.op("dve", lambda e, ps=ps, g4=g4: e.tensor_scalar(out=negc[:, g4 * 4:(g4 + 1) * 4, :], in0=ps[:, 0:32].rearrange("p (j h) -> p j h", j=4),
                                                                    scalar1=-1.0, scalar2=None, op0=ALU.mult), reads=[pk], writes=["negc"])
            fence(R2KEYS)
            if stop_after == "logf":
                S.emit(finals)
                return nc

            qTm = R2[:, 0:2080]
            kTm = R2[:, 2080:4160]
            Vp = R2[:, 4160:4160 + 17 * 256].rearrange("p (t h c) -> p t h c", t=17, h=2)
            attnT = R2[:, 8512:8512 + 4 * NTOK].rearrange("p (c n) -> p c n", c=4)
            PT = [R2[:, 16832 + i * 512:16832 + (i + 1) * 512] for i in range(3)]
            S.op("dve", lambda e: e.memset(R2[:, 4160:4160 + 17 * 256], 0.0), writes=["Vp"])
            S.op("dve", lambda e: e.memset(qTm[64:128, :], 0.0), writes=["qTs"])
            S.op("dve", lambda e: e.memset(kTm[64:128, :], 0.0), writes=["kTs"])
            S.dma("sp", qTm[64:72, 0:T], c8b[:, :], reads=["c8b"], writes=["qTs"])
            psrot[0] = [0, 1, 2]
            fence(DKEYS + RSKEYS)
            dg = decode_gen(l, attnT)

            def dstep(n=1):
                for _ in range(n):
                    next(dg, None)
            dstep()
            stgi = [0]

            def stage_out(ps_ap, pk, np_, ncol, dram_ap, after):
                si = stgi[0] % 2
                stgi[0] += 1
                S.op("act", lambda e: e.copy(out=stg[si][0:np_, 0:ncol], in_=ps_ap), reads=[pk], writes=["stg%d" % si], deps=[after])
                finals.append(S.dma("sp", dram_ap, stg[si][0:np_, 0:ncol], reads=["stg%d" % si]))

            def qkv_slab(hp_):
                return slab([(0, KC, 128, wview(w_in_l, OQ + hp_ * 128, 128)),
                             (1024, KC, 128, wview(w_in_l, OK_ + hp_ * 128, 128)),
                             (2048, KC, 128, wview(w_in_l, OV + hp_ * 128, 128))])
            nxt = qkv_slab(0)
            for hp in range(4):
                wq, kq = nxt
                if hp < 3:
                    nxt = qkv_slab(hp + 1)
                wq3, wk3, wv3 = w3(wq, KC, 128, 0), w3(wq, KC, 128, 1024), w3(wq, KC, 128, 2048)
                for tt in range(17):
                    r0, nr = tt * 128, (128 if tt < 16 else NS)
                    t = min(tt // 4, 4)
                    ps, pk = ps_gen()
                    proj(ps[0:nr, 0:128], pk, lambda k: xnT[:, k, r0:r0 + nr], KC, lambda k: wv3[:, k, :], xnkeys(t), kq)
                    S.op("act", lambda e, ps=ps, tt=tt, nr=nr: e.copy(out=Vp[0:nr, tt, 0, 0:64], in_=ps[0:nr, 0:64]), reads=[pk], writes=["Vp"])
                    o_ = S.op("act", lambda e, ps=ps, tt=tt, nr=nr: e.copy(out=Vp[0:nr, tt, 1, 64:128], in_=ps[0:nr, 64:128]), reads=[pk], writes=["Vp"])
                    stage_out(ps[0:nr, 0:128], pk, nr, 128, v_o[l, r0:r0 + nr, hp * 128:(hp + 1) * 128], o_)
                    dstep()
                for hh in range(2):
                    h = 2 * hp + hh
                    S.dma("pool", kTm[64:72, 0:T], oh_d[h], writes=["kTs", "kTb"])
                    for t, (c0, w) in enumerate(TILES):
                        ps, pk = ps_gen()
                        proj(ps[0:64, 0:w], pk, lambda k: wq3[:, k, hh * 64:(hh + 1) * 64], KC, lambda k: xnT[:, k, c0:c0 + w], xnkeys(t), kq)
                        S.op("act", lambda e, ps=ps, c0=c0, w=w: e.copy(out=qTm[0:64, c0:c0 + w], in_=ps[0:64, 0:w]), reads=[pk], writes=["qT"])
                        ps, pk = ps_gen()
                        proj(ps[0:64, 0:w], pk, lambda k: wk3[:, k, hh * 64:(hh + 1) * 64], KC, lambda k: xnT[:, k, c0:c0 + w], xnkeys(t), kq)
                        o_ = S.op("act", lambda e, ps=ps, c0=c0, w=w: e.copy(out=kTm[0:64, c0:c0 + w], in_=ps[0:64, 0:w]), reads=[pk], writes=["kTb"])
                        stage_out(ps[0:64, 0:w], pk, 64, w, kT_o[l, h * 64:(h + 1) * 64, c0:c0 + w], o_)
                        dstep()
                    for qi in range(4):
                        q0 = qi * 512
                        pO, pS = psb[6], psb[7]
                        nkj = 4 * qi + 4
                        pend = None
                        for kj in range(nkj):
                            r = kj - 4 * qi
                            cs = max(r, 0) * 128
                            bank = 4 + (kj % 2)
                            pst, pkt = psb[bank], "ps%d" % bank
                            S.op("pe", lambda e, pst=pst, kj=kj, cs=cs, r=r: e.matmul(
                                pst[:, cs:512], lhsT=kTm[:, kj * 128:(kj + 1) * 128], rhs=qTm[:, q0 + cs:q0 + 512], start=True, stop=(r < 0)),
                                reads=["qT", "kTb", "qTs", "kTs"], writes=[pkt])
                            if r >= 0:
                                S.op("pe", lambda e, pst=pst, cs=cs: e.matmul(pst[:, cs:cs + 128], lhsT=ident, rhs=mneg, start=False, stop=True),
                                     reads=["cst"], writes=[pkt])
                            pt_i = kj % 3
                            S.op("act", lambda e, pst=pst, pt_i=pt_i, kj=kj, h=h, cs=cs: e.activation(
                                out=PT[pt_i][:, cs:512], in_=pst[:, cs:512], func=AF.Exp, scale=0.125, bias=negc[:, kj, h:h + 1]),
                                reads=[pkt, "negc"], writes=["PT%d" % pt_i])
                            dstep()
                            cur = (pt_i, kj, hh, cs, kj)
                            if pend is not None:
                                _pv(S, pend, PT, Vp, opad, pO, pS, nkj)
                            pend = cur
                        _pv(S, pend, PT, Vp, opad, pO, pS, nkj)
                        si = stgi[0] % 2
                        stgi[0] += 1
                        rc = stg[si]
                        lo, hi = hh * 64, (hh + 1) * 64
                        S.op("dve", lambda e, rc=rc, lo=lo, hi=hi: e.reciprocal(out=rc[lo:hi, :], in_=pS[lo:hi, :]), reads=["ps7"], writes=["stg%d" % si])
                        S.op("dve", lambda e, rc=rc, q0=q0, hp=hp, lo=lo, hi=hi: e.tensor_tensor(out=attnT[lo:hi, hp, q0:q0 + 512], in0=pO[lo:hi, :], in1=rc[lo:hi, :], op=ALU.mult),
                             reads=["ps6", "stg%d" % si], writes=["attn%d" % hp])
            for _ in dg:
                pass
            psrot[0] = list(range(8))
            fence(["qT", "kTb", "qTs", "kTs", "Vp", "PT0", "PT1", "PT2", "t1", "t2", "t3", "xcb", "sgt", "sga", "tmpm"] + R1KEYS)

            if stop_after == "attn":
                S.emit(finals)
                return nc
            yTt = w3(R1, KC, 512, 0)
            mrg = w3(R1, KC, 512, 4096)
            t1, t2, t3 = [r2f(i * 1056, 528) for i in range(3)]
            xcb = R2[:, 3168:3680]
            rsb2 = rstd[:, :].bitcast(BF16)
            tsets = [(t1, t2, t3, xcb),
                     tuple(rsb2[:, i * 1056:(i + 1) * 1056].bitcast(F32) for i in range(3)) + (rsb2[:, 3168:3680],)]
            tkeys = [("t1", "t2", "t3", "xcb"), ("t1b", "t2b", "t3b", "xcbb")]
            fence(DKEYS + RSKEYS + list(tkeys[1]))
            sgt = r2f(3680, 512)
            sga = r2f(4704, 512)
            tmpm = r2f(5728, 512)
            S.op("dve", lambda e: e.memset(carry[:], 0.0), writes=["carry%d" % n_ for n_ in range(KC)])
            S.op("dve", lambda e: e.memset(hcar[:], 0.0), writes=["hcar%d" % n_ for n_ in range(KC)])
            S.dma("sp", prevS[:], scT_d[l].rearrange("(k p) j b -> p k j b", p=128), writes=["prevS"])
            S.dma("sp", h0S[:], shT_d[l].rearrange("(k p) b -> p k b", p=128), writes=["h0S"])
            for t, (c0, w) in enumerate(TILES):
                samp = (t == 4)
                for half in range(2):
                    wxr, kxr = slab([(0, KC, 512, wview(w_in_l, OXR + half * 512, 512))])
                    wgr, kgr = slab([(0, KC, 512, wview(w_in_l, OGR + half * 512, 512))])
                    wxr3, wgr3 = w3(wxr, KC), w3(wgr, KC)
                    for nn in range(4):
                        n = half * 4 + nn
                        t1, t2, t3, xcb = tsets[n % 2]
                        kt1, kt2, kt3, kxc = tkeys[n % 2]
                        ps, pk = ps_gen()
                        proj(ps[:, 0:w], pk, lambda k: wxr3[:, k, nn * 128:(nn + 1) * 128], KC, lambda k: xnT[:, k, c0:c0 + w], xnkeys(t), kxr)
                        cw = [vcol(l, V_CW + j * 8, n) for j in range(4)]
                        cb = vcol(l, V_CB, n)
                        if not samp:
                            S.op("dve", lambda e, n=n: e.tensor_copy(out=t1[:, 0:3], in_=carry[:, n, :]), reads=["carry%d" % n], writes=[kt1])
                            S.op("act", lambda e, ps=ps, w=w: e.copy(out=t1[:, 3:3 + w], in_=ps[:, 0:w]), reads=[pk], writes=[kt1])
                            S.op("dve", lambda e, n=n, w=w: e.tensor_copy(out=carry[:, n, :], in_=t1[:, w:w + 3]), reads=[kt1], writes=["carry%d" % n])
                            S.op("dve", lambda e, w=w, cw=cw, cb=cb: e.tensor_scalar(out=t2[:, 0:w], in0=t1[:, 3:3 + w], scalar1=cw[3], scalar2=cb, op0=ALU.mult, op1=ALU.add),
                                 reads=[kt1, "vecs"], writes=[kt2])
                            for j in range(3):
                                S.op("dve", lambda e, j=j, w=w, cw=cw: e.scalar_tensor_tensor(out=t2[:, 0:w], in0=t1[:, j:j + w], scalar=cw[j], in1=t2[:, 0:w], op0=ALU.mult, op1=ALU.add),
                                     reads=[kt1, kt2, "vecs"], writes=[kt2])
                        else:
                            S.op("act", lambda e, ps=ps, w=w: e.copy(out=t1[:, 0:w], in_=ps[:, 0:w]), reads=[pk], writes=[kt1])
                            S.op("dve", lambda e, w=w, cw=cw, cb=cb: e.tensor_scalar(out=t2[:, 0:w], in0=t1[:, 0:w], scalar1=cw[3], scalar2=cb, op0=ALU.mult, op1=ALU.add),
                                 reads=[kt1, "vecs"], writes=[kt2])
                            for j in range(3):
                                S.op("dve", lambda e, j=j, n=n, w=w, cw=cw: e.scalar_tensor_tensor(out=t2[:, 0:w], in0=prevS[:, n, j, :], scalar=cw[j], in1=t2[:, 0:w], op0=ALU.mult, op1=ALU.add),
                                     reads=["prevS", kt2, "vecs"], writes=[kt2])
                            S.op("dve", lambda e, n=n, w=w: e.tensor_copy(out=xrS[:, n, :], in_=t1[:, 0:w]), reads=[kt1], writes=["xrS"])
                        S.op("dve", lambda e, w=w: e.tensor_copy(out=xcb[:, 0:w], in_=t2[:, 0:w]), reads=[kt2], writes=[kxc])
                        psr, pkr = ps_gen()
                        S.op("pe", lambda e, psr=psr, n=n, w=w: e.matmul(psr[:, 0:w], lhsT=wgate[:, 0, n, :], rhs=xcb[:, 0:w], start=True, stop=True), reads=["wgate", kxc], writes=[pkr])
                        psi_, pki = ps_gen()
                        S.op("pe", lambda e, psi_=psi_, n=n, w=w: e.matmul(psi_[:, 0:w], lhsT=wgate[:, 1, n, :], rhs=xcb[:, 0:w], start=True, stop=True), reads=["wgate", kxc], writes=[pki])
                        S.op("act", lambda e, psr=psr, n=n, w=w: e.activation(out=t1[:, 0:w], in_=psr[:, 0:w], func=AF.Sigmoid, bias=vcol(l, V_BRG, n)), reads=[pkr, "vecs"], writes=[kt1])
                        S.op("act", lambda e, psi_=psi_, n=n, w=w: e.activation(out=t3[:, 0:w], in_=psi_[:, 0:w], func=AF.Sigmoid, bias=vcol(l, V_BIG, n)), reads=[pki, "vecs"], writes=[kt3])
                        S.op("act", lambda e, n=n, w=w: e.activation(out=t1[:, 0:w], in_=t1[:, 0:w], func=AF.Exp, scale=lsc[:, l * KC + n:l * KC + n + 1]), reads=[kt1, "lsc"], writes=[kt1])
                        S.op("dve", lambda e, w=w: e.tensor_tensor(out=t3[:, 0:w], in0=t3[:, 0:w], in1=t2[:, 0:w], op=ALU.mult), reads=[kt3, kt2], writes=[kt3])
                        S.op("dve", lambda e, w=w: e.tensor_tensor(out=t2[:, 0:w], in0=t1[:, 0:w], in1=t1[:, 0:w], op=ALU.mult), reads=[kt1], writes=[kt2])
                        S.op("act", lambda e, w=w: e.activation(out=t2[:, 0:w], in_=t2[:, 0:w], func=AF.Sqrt, scale=-1.0, bias=1.0), reads=[kt2], writes=[kt2])
                        S.op("dve", lambda e, w=w: e.tensor_tensor(out=t3[:, 0:w], in0=t3[:, 0:w], in1=t2[:, 0:w], op=ALU.mult), reads=[kt3, kt2], writes=[kt3])
                        if not samp:
                            S.op("dve", lambda e, n=n, w=w: e.tensor_tensor_scan(out=t2[:, 0:w], data0=t1[:, 0:w], data1=t3[:, 0:w], initial=hcar[:, n:n + 1], op0=ALU.mult, op1=ALU.add),
                                 reads=[kt1, kt3, "hcar%d" % n], writes=[kt2])
                            S.op("dve", lambda e, n=n, w=w: e.tensor_copy(out=hcar[:, n:n + 1], in_=t2[:, w - 1:w]), reads=[kt2], writes=["hcar%d" % n])
                        else:
                            S.op("dve", lambda e, n=n, w=w: e.tensor_tensor(out=t2[:, 0:w], in0=t1[:, 0:w], in1=h0S[:, n, :], op=ALU.mult), reads=[kt1, "h0S"], writes=[kt2])
                            S.op("dve", lambda e, w=w: e.tensor_tensor(out=t2[:, 0:w], in0=t2[:, 0:w], in1=t3[:, 0:w], op=ALU.add), reads=[kt2, kt3], writes=[kt2])
                            S.op("dve", lambda e, n=n, w=w: e.tensor_copy(out=hS[:, n, 1:1 + w], in_=t2[:, 0:w]), reads=[kt2], writes=["hS"])
                        psg, pkg = ps_gen()
                        proj(psg[:, 0:w], pkg, lambda k: wgr3[:, k, nn * 128:(nn + 1) * 128], KC, lambda k: xnT[:, k, c0:c0 + w], xnkeys(t), kgr)
                        S.op("act", lambda e, psg=psg, w=w: e.activation(out=t3[:, 0:w], in_=psg[:, 0:w], func=AF.Gelu_apprx_tanh), reads=[pkg], writes=[kt3])
                        S.op("dve", lambda e, n=n, w=w: e.tensor_tensor(out=yTt[:, n, 0:w], in0=t2[:, 0:w], in1=t3[:, 0:w], op=ALU.mult), reads=[kt2, kt3], writes=["y%d" % n])
                for half in range(2):
                    wb, kb = slab([(0, KC, 512, wview(w_b_d[l], half * 512, 512))])
                    wgb, kgb = slab([(0, KC, 512, wview(w_in_l, OGB + half * 512, 512))])
                    wb3, wgb3 = w3(wb, KC), w3(wgb, KC)
                    for mm in range(4):
                        m = half * 4 + mm
                        psB, pkB = ps_gen()
                        proj(psB[:, 0:w], pkB, lambda k: wb3[:, k, mm * 128:(mm + 1) * 128], KC, lambda k: yTt[:, k, 0:w], ["y%d" % k for k in range(KC)], kb)
                        psG, pkG = ps_gen()
                        proj(psG[:, 0:w], pkG, lambda k: wgb3[:, k, mm * 128:(mm + 1) * 128], KC, lambda k: xnT[:, k, c0:c0 + w], xnkeys(t), kgb)
                        S.op("act", lambda e, psG=psG, w=w: e.activation(out=sgt[:, 0:w], in_=psG[:, 0:w], func=AF.Sigmoid), reads=[pkG], writes=["sgt"])
                        S.op("dve", lambda e, psB=psB, m=m, w=w: e.tensor_tensor(out=mrg[:, m, 0:w], in0=psB[:, 0:w], in1=sgt[:, 0:w], op=ALU.mult), reads=[pkB, "sgt"], writes=["mg%d" % m])
                for half in range(2):
                    wa, ka = slab([(0, 4, 512, wview(w_a_d[l], half * 512, 512))])
                    wga, kga = slab([(0, KC, 512, wview(w_in_l, OGA + half * 512, 512))])
                    wa3, wga3 = w3(wa, 4), w3(wga, KC)
                    for mm in range(4):
                        m = half * 4 + mm
                        psA, pkA = ps_gen()
                        akeys = ["attn%d" % k for k in range(4)] if not samp else ["attnS"] * 4
                        proj(psA[:, 0:w], pkA, lambda k: wa3[:, k, mm * 128:(mm + 1) * 128], 4, lambda k: attnT[:, k, c0:c0 + w], akeys, ka)
                        psG, pkG = ps_gen()
                        proj(psG[:, 0:w], pkG, lambda k: wga3[:, k, mm * 128:(mm + 1) * 128], KC, lambda k: xnT[:, k, c0:c0 + w], xnkeys(t), kga)
                        S.op("act", lambda e, psG=psG, w=w: e.activation(out=sga[:, 0:w], in_=psG[:, 0:w], func=AF.Sigmoid), reads=[pkG], writes=["sga"])
                        S.op("dve", lambda e, psA=psA, w=w: e.tensor_tensor(out=tmpm[:, 0:w], in0=psA[:, 0:w], in1=sga[:, 0:w], op=ALU.mult), reads=[pkA, "sga"], writes=["tmpm"])
                        S.op("dve", lambda e, m=m, w=w: e.tensor_tensor(out=mrg[:, m, 0:w], in0=tmpm[:, 0:w], in1=mrg[:, m, 0:w], op=ALU.add), reads=["tmpm", "mg%d" % m], writes=["mg%d" % m])
                for half in range(2):
                    wo, ko = slab([(0, KC, 512, wview(w_o_d[l], half * 512, 512))])
                    wo3 = w3(wo, KC)
                    for mm in range(4):
                        m = half * 4 + mm
                        ps, pk = ps_gen()
                        proj(ps[:, 0:w], pk, lambda k: wo3[:, k, mm * 128:(mm + 1) * 128], KC, lambda k: mrg[:, k, 0:w], ["mg%d" % k for k in range(KC)], ko)
                        evac_add_x(ps, pk, t, m, c0, w)
            S.op("dve", lambda e: e.tensor_copy(out=hS[:, :, 0:1], in_=hcar[:].unsqueeze(2)), reads=["hcar%d" % n_ for n_ in range(KC)], writes=["hS"])
            finals.append(S.dma("sp", cvT_o[l].rearrange("(k p) j -> p k j", p=128), carry[:], reads=["carry%d" % n_ for n_ in range(KC)]))
            cvo = cvS_o[l].rearrange("(k p) j b -> p k j b", p=128)
            finals.append(S.dma("sp", cvo[:, :, 0:2, :], prevS[:, :, 1:3, :], reads=["prevS"]))
            finals.append(S.dma("sp", cvo[:, :, 2, :], xrS[:], reads=["xrS"]))
            finals.append(S.dma("sp", hT_o[l].rearrange("(k p) b -> p k b", p=128), hS[:], reads=["hS"]))

            if stop_after == "mix":
                S.emit(finals)
                return nc
            fence(DKEYS + RSKEYS + ["t1b", "t2b", "t3b", "xcbb"])
            rmsnorm(l, V_GFFN)
            fence(R2KEYS + R1KEYS)
            hT = w3(R1, 4, NTOK)
            sgf = r2f(0, 512)
            tf = r2f(1024, 512)
            Ge = r2f(2048, NTOK)

            def ffn_pass(wg2d, wu2d, wd2d, gate):
                for fg in range(FCH // 4):
                    wg, kg = slab([(0, KC, 512, wview(wg2d, fg * 512, 512))])
                    wu, ku = slab([(0, KC, 512, wview(wu2d, fg * 512, 512))])
                    wg3, wu3 = w3(wg, KC), w3(wu, KC)
                    for ff in range(4):
                        for t, (c0, w) in enumerate(TILES):
                            psg, pkg = ps_gen()
                            proj(psg[:, 0:w], pkg, lambda k: wg3[:, k, ff * 128:(ff + 1) * 128], KC, lambda k: xnT[:, k, c0:c0 + w], xnkeys(t), kg)
                            psu, pku = ps_gen()
                            proj(psu[:, 0:w], pku, lambda k: wu3[:, k, ff * 128:(ff + 1) * 128], KC, lambda k: xnT[:, k, c0:c0 + w], xnkeys(t), ku)
                            S.op("act", lambda e, psg=psg, w=w: e.activation(out=sgf[:, 0:w], in_=psg[:, 0:w], func=AF.Silu), reads=[pkg], writes=["sgf"])
                            hk = "h%d_%d" % (ff, t)
                            if not gate:
                                S.op("dve", lambda e, psu=psu, ff=ff, c0=c0, w=w: e.tensor_tensor(out=hT[:, ff, c0:c0 + w], in0=psu[:, 0:w], in1=sgf[:, 0:w], op=ALU.mult),
                                     reads=[pku, "sgf"], writes=[hk])
                            else:
                                S.op("dve", lambda e, psu=psu, w=w: e.tensor_tensor(out=tf[:, 0:w], in0=psu[:, 0:w], in1=sgf[:, 0:w], op=ALU.mult), reads=[pku, "sgf"], writes=["tf"])
                                S.op("dve", lambda e, ff=ff, c0=c0, w=w: e.tensor_tensor(out=hT[:, ff, c0:c0 + w], in0=tf[:, 0:w], in1=Ge[:, c0:c0 + w], op=ALU.mult),
                                     reads=["tf", "Ge"], writes=[hk])
                    wd, kd = slab([(0, 4, 1024, wd2d[fg * 512:(fg + 1) * 512, :].rearrange("(k p) n -> p k n", p=128))])
                    wd3 = w3(wd, 4, 1024)
                    for m in range(KC):
                        for t, (c0, w) in enumerate(TILES):
                            ps, pk = ps_gen()
                            proj(ps[:, 0:w], pk, lambda k: wd3[:, k, m * 128:(m + 1) * 128], 4, lambda k: hT[:, k, c0:c0 + w], ["h%d_%d" % (k, t) for k in range(4)], kd)
                            evac_add_x(ps, pk, t, m, c0, w)

            if l % 2 == 0:
                ffn_pass(dwg_d, dwu_d, dwd_d, False)
            else:
                S.dma("sp", rtw[:], rt_d.rearrange("(k p) n -> p k n", p=128), writes=["rtw"])
                xnf = r2f(2048, NTOK)
                lgS = r2f(6208, NTOK)
                e8f = r2f(10368, 1024)[0:NEXP]
                gwk = r2f(12416, 816).rearrange("p (a b c) -> p a b c", a=6, b=17)
                lgt = r2f(14048, 136).rearrange("p (b c) -> p b c", b=17)
                m12 = r2f(14320, 68).rearrange("p (a b) -> p a b", a=4)
                S.dma("sp", e8f, e8_d, writes=["e8f"])
                for k in range(KC):
                    for t, (c0, w) in enumerate(TILES):
                        S.op("dve", lambda e, k=k, c0=c0, w=w: e.scalar_tensor_tensor(out=xnf[:, c0:c0 + w], in0=xT[:, k, c0:c0 + w], scalar=vcol(l, V_GFFN, k),
                                                                                        in1=rstd[:, c0:c0 + w], op0=ALU.mult, op1=ALU.mult),
                             reads=[xk(t, k), "rstd%d" % t, "vecs"], writes=["xnf%d" % t])
                    for t, (c0, w) in enumerate(TILES):
                        S.op("pe", lambda e, k=k, t=t, c0=c0, w=w: e.matmul(_lg_ps(psb, t)[0:NEXP, 0:w], lhsT=rtw[:, k, :], rhs=xnf[:, c0:c0 + w],
                                                                            start=(k == 0), stop=(k == KC - 1)),
                             reads=["rtw", "xnf%d" % t], writes=[_lg_key(t)])
                for t, (c0, w) in enumerate(TILES):
                    S.op("dve", lambda e, t=t, c0=c0, w=w: e.tensor_copy(out=lgS[0:NEXP, c0:c0 + w], in_=_lg_ps(psb, t)[0:NEXP, 0:w]), reads=[_lg_key(t)], writes=["lgS"])
                for g5 in range(5):
                    ps, pk = ps_gen()
                    nb = 4 if g5 < 4 else 1
                    for j in range(nb):
                        tt = g5 * 4 + j
                        r0, nr = tt * 128, (128 if tt < 16 else NS)
                        S.op("pe", lambda e, ps=ps, j=j, r0=r0, nr=nr: e.matmul(ps[0:nr, j * 8:(j + 1) * 8], lhsT=lgS[0:NEXP, r0:r0 + nr], rhs=identf[0:NEXP, 0:NEXP], start=True, stop=True),
                             reads=["lgS", "identf"], writes=[pk])
                    if g5 == 4:
                        S.op("dve", lambda e: e.memset(lgt[:, 16, :], 0.0), writes=["lgt"])
                        S.op("dve", lambda e, ps=ps: e.tensor_copy(out=lgt[0:NS, 16, :], in_=ps[0:NS, 0:8]), reads=[pk], writes=["lgt"])
                    else:
                        S.op("dve", lambda e, ps=ps, g5=g5: e.tensor_copy(out=lgt[:, g5 * 4:(g5 + 1) * 4, :], in_=ps[:, 0:32].rearrange("p (j h) -> p j h", j=4)), reads=[pk], writes=["lgt"])
                m1, m2, dd, w1 = m12[:, 0], m12[:, 1], m12[:, 2], m12[:, 3]
                eq1, lg2, eq2, g1, g2, gt = [gwk[:, i] for i in range(6)]
                bc = lambda a: a.unsqueeze(2).broadcast_to([128, 17, NEXP])
                S.op("dve", lambda e: e.tensor_reduce(out=m1, in_=lgt, axis=AX.X, op=ALU.max), reads=["lgt"], writes=["m12"])
                S.op("dve", lambda e: e.tensor_tensor(out=eq1, in0=lgt, in1=bc(m1), op=ALU.is_equal), reads=["lgt", "m12"], writes=["gwk"])
                S.op("dve", lambda e: e.scalar_tensor_tensor(out=lg2, in0=eq1, scalar=-1e30, in1=lgt, op0=ALU.mult, op1=ALU.add), reads=["gwk", "lgt"], writes=["gwk"])
                S.op("dve", lambda e: e.tensor_reduce(out=m2, in_=lg2, axis=AX.X, op=ALU.max), reads=["gwk"], writes=["m12"])
                S.op("dve", lambda e: e.tensor_tensor(out=eq2, in0=lg2, in1=bc(m2), op=ALU.is_equal), reads=["gwk", "m12"], writes=["gwk"])
                S.op("dve", lambda e: e.tensor_tensor(out=dd, in0=m2, in1=m1, op=ALU.subtract), reads=["m12"], writes=["m12"])
                S.op("act", lambda e: e.activation(out=dd, in_=dd, func=AF.Exp), reads=["m12"], writes=["m12"])
                S.op("dve", lambda e: e.tensor_scalar(out=w1, in0=dd, scalar1=1.0, scalar2=None, op0=ALU.add), reads=["m12"], writes=["m12"])
                S.op("dve", lambda e: e.reciprocal(out=w1, in_=w1), reads=["m12"], writes=["m12"])
                S.op("dve", lambda e: e.tensor_tensor(out=dd, in0=dd, in1=w1, op=ALU.mult), reads=["m12"], writes=["m12"])
                S.op("dve", lambda e: e.tensor_tensor(out=g1, in0=eq1, in1=bc(w1), op=ALU.mult), reads=["gwk", "m12"], writes=["gwk"])
                S.op("dve", lambda e: e.tensor_tensor(out=g2, in0=eq2, in1=bc(dd), op=ALU.mult), reads=["gwk", "m12"], writes=["gwk"])
                S.op("dve", lambda e: e.tensor_tensor(out=gt, in0=g1, in1=g2, op=ALU.add), reads=["gwk"], writes=["gwk"])
                fence(["lgS", "gatesT"])
                gatesT = lgS[0:NEXP]
                for tt in range(17):
                    r0, nr = tt * 128, (128 if tt < 16 else NS)
                    ps, pk = ps_gen()
                    S.op("pe", lambda e, ps=ps, tt=tt, nr=nr: e.matmul(ps[0:NEXP, 0:nr], lhsT=gt[0:nr, tt, :], rhs=identf[0:nr, 0:nr], start=True, stop=True),
                         reads=["gwk", "identf"], writes=[pk])
                    S.op("dve", lambda e, ps=ps, r0=r0, nr=nr: e.tensor_copy(out=gatesT[:, r0:r0 + nr], in_=ps[0:NEXP, 0:nr]), reads=[pk], writes=["gatesT"])
                fence(["Ge"] + ["xnf%d" % t for t in range(5)])
                for ex in range(NEXP):
                    for t, (c0, w) in enumerate(TILES):
                        ps, pk = ps_gen()
                        S.op("pe", lambda e, ps=ps, ex=ex, c0=c0, w=w: e.matmul(ps[:, 0:w], lhsT=e8f[:, ex * 128:(ex + 1) * 128], rhs=gatesT[:, c0:c0 + w], start=True, stop=True),
                             reads=["e8f", "gatesT"], writes=[pk])
                        S.op("act", lambda e, ps=ps, c0=c0, w=w: e.copy(out=Ge[:, c0:c0 + w], in_=ps[:, 0:w]), reads=[pk], writes=["Ge"])
                    ffn_pass(mwg_d[ex], mwu_d[ex], mwd_d[ex], True)

            if stop_after == "ffn":
                S.emit(finals)
                return nc
            rmsnorm(l, V_GPLE)
            fence(R2KEYS)
            pTb = w3(R2, 2, NTOK, 2048)
            pv_ = pT_d[l].rearrange("(k p) n -> p k n", p=128)
            for (ca, cb_) in ((0, 1026), (1026, NTOK)):
                S.dma("pool", pTb[:, :, ca:cb_], pv_[:, :, ca:cb_], writes=["pTb"])
            sgp = r2f(0, 512)
            tp = r2f(1024, 512)
            for half in range(2):
                wpg, kpg = slab([(0, KC, 512, wview(wpg_d[l], half * 512, 512))])
                wpp, kpp = slab([(0, 2, 512, wview(wpp_d[l], half * 512, 512))])
                wpg3, wpp3 = w3(wpg, KC), w3(wpp, 2)
                for mm in range(4):
                    m = half * 4 + mm
                    for t, (c0, w) in enumerate(TILES):
                        psG, pkG = ps_gen()
                        proj(psG[:, 0:w], pkG, lambda k: wpg3[:, k, mm * 128:(mm + 1) * 128], KC, lambda k: xnT[:, k, c0:c0 + w], xnkeys(t), kpg)
                        psP, pkP = ps_gen()
                        proj(psP[:, 0:w], pkP, lambda k: wpp3[:, k, mm * 128:(mm + 1) * 128], 2, lambda k: pTb[:, k, c0:c0 + w], ["pTb", "pTb"], kpp)
                        S.op("act", lambda e, psG=psG, w=w: e.activation(out=sgp[:, 0:w], in_=psG[:, 0:w], func=AF.Sigmoid), reads=[pkG], writes=["sgp"])
                        S.op("dve", lambda e, psP=psP, w=w: e.tensor_tensor(out=tp[:, 0:w], in0=psP[:, 0:w], in1=sgp[:, 0:w], op=ALU.mult), reads=[pkP, "sgp"], writes=["tp"])
                        S.op("dve", lambda e, m=m, c0=c0, w=w: e.tensor_tensor(out=xT[:, m, c0:c0 + w], in0=tp[:, 0:w], in1=xT[:, m, c0:c0 + w], op=ALU.add),
                             reads=["tp", xk(t, m)], writes=[xk(t, m)])

        fi = [0]

        def fin(t, k, c0, w, g, rk):
            si = fi[0] % 2
            fi[0] += 1
            S.op("dve", lambda e: e.scalar_tensor_tensor(out=stg[si][:, 0:w], in0=xT[:, k, c0:c0 + w], scalar=g, in1=rstd[:, c0:c0 + w], op0=ALU.mult, op1=ALU.mult),
                 reads=[xk(t, k), rk, "vecs"], writes=["stg%d" % si])
            finals.append(S.dma("sp", yT_o[k * 128:(k + 1) * 128, c0:c0 + w], stg[si][:, 0:w], reads=["stg%d" % si]))

        rmsnorm(None, V_GFIN, fp32_out=fin)
        S.emit(finals)
    return nc


def _lg_ps(psb, t):
    return psb[4 + t] if t < 4 else psb[3]


def _lg_key(t):
    return "ps%d" % (4 + t) if t < 4 else "ps3"


def _pv(S, cur, PT, Vp, opad, pO, pS, nkj):
    pt_i, kj, hh, cs, cnt = cur
    first = (cnt == 0)
    lastg = (cnt == nkj - 1)
    S.op("pe", lambda e: e.matmul(pO[:, cs:512], lhsT=Vp[:, kj, hh, :], rhs=PT[pt_i][:, cs:512], start=first, stop=lastg),
         reads=["PT%d" % pt_i, "Vp"], writes=["ps6"])
    S.op("pe", lambda e: e.matmul(pS[:, cs:512], lhsT=opad[hh], rhs=PT[pt_i][:, cs:512], start=first, stop=lastg),
         reads=["PT%d" % pt_i, "cst"], writes=["ps7"])


def _constants():
    cst = np.zeros((128, 5 * 128), np.float32)
    cst[:, 0:128] = np.eye(128, dtype=np.float32)
    cst[:, 128:256] = 1.0
    kk, qq = np.meshgrid(np.arange(128), np.arange(128), indexing="ij")
    cst[:, 256:384] = np.where(kk > qq, -30000.0, 0.0)
    cst[:, 384:384 + 64] = 1.0
    cst[:, 512 + 64:512 + 128] = 1.0
    e8 = np.zeros((8, 8, 128), np.float32)
    for h in range(8):
        e8[h, h, :] = 1.0
    return cst, e8.reshape(8, 8 * 128)


def _onehot_rows():
    oh = np.zeros((NH, 8, T), np.float32)
    for h in range(NH):
        oh[h, h, :] = 1.0
    return oh


_NC_CACHE = {}
LAST_SCHED = None


def _decode_consts(b_f):
    cf = np.zeros((128, 256 + 2 * NH + 1), np.float32)
    ii, jj = np.meshgrid(np.arange(128), np.arange(128), indexing="ij")
    cf[:, 0:128] = (ii > jj).astype(np.float32)
    cf[:, 128:256] = 1.0
    cf[:, 256:256 + 2 * NH] = np.asarray(b_f, np.float32).reshape(1, 2 * NH)
    cf[:, 256 + 2 * NH] = np.arange(128, dtype=np.float32)
    return cf


def kernel(x_prompt, x_sample, p_prompt, p_sample, cache_k, cache_v, cache_logf, state_conv, state_h,
           page_table, g_mix, w_in, b_f, conv_w, conv_b, w_rg, b_rg, w_ig, b_ig, lru_lambda,
           w_a_up, w_b_up, w_out, g_ffn, dense_wg, dense_wu, dense_wd, moe_router, moe_wg, moe_wu,
           moe_wd, g_ple, w_ple_gate, w_ple_proj, g_final):
    f = lambda a: np.ascontiguousarray(np.asarray(a, dtype=np.float32))
    x_prompt, x_sample, p_prompt, p_sample = f(x_prompt), f(x_sample), f(p_prompt), f(p_sample)
    state_conv, state_h = f(state_conv), f(state_h)
    page_table = np.ascontiguousarray(np.asarray(page_table, dtype=np.int32))
    vecs = np.zeros((128, NV), np.float32)

    def put(base, v):
        vecs[:, base:base + 8] = f(v).reshape(8, 128).T

    for l in range(DEPTH):
        b = l * LV
        put(b + V_GMIX, g_mix[l])
        for j in range(4):
            put(b + V_CW + 8 * j, conv_w[l][j])
        put(b + V_CB, conv_b[l]); put(b + V_BRG, b_rg[l]); put(b + V_BIG, b_ig[l])
        put(b + V_LAM, lru_lambda[l]); put(b + V_GFFN, g_ffn[l]); put(b + V_GPLE, g_ple[l])
    put(V_GFIN, g_final)
    cst, e8 = _constants()
    shared = dict(
        vecs=vecs, bfT=f(f(b_f).T), w_in=f(w_in), w_rg=f(w_rg), w_ig=f(w_ig), w_a_up=f(w_a_up), w_b_up=f(w_b_up),
        w_out=f(w_out), dense_wg=f(dense_wg)[0], dense_wu=f(dense_wu)[0], dense_wd=f(dense_wd)[0],
        router=f(moe_router)[0], moe_wg=f(moe_wg)[0], moe_wu=f(moe_wu)[0], moe_wd=f(moe_wd)[0],
        w_ple_gate=f(w_ple_gate), w_ple_proj=f(w_ple_proj), cst=cst, e8=e8, oh=_onehot_rows(),
        ck=f(cache_k).reshape(DEPTH, NPOOL, 128, 512), cv=f(cache_v).reshape(DEPTH, NPOOL, 128, 512),
        clf=f(cache_logf), cf=_decode_consts(b_f))
    in_maps = []
    for c in range(NCORES):
        sl = slice(c * NS, (c + 1) * NS)
        m = dict(shared)
        m["xT"] = f(np.concatenate([x_prompt[c].T, x_sample[sl, 0, :].T], axis=1))
        m["pT"] = f(np.stack([np.concatenate([p_prompt[l, c].T, p_sample[l, sl, 0, :].T], axis=1) for l in range(DEPTH)]))
        m["scT"] = f(np.transpose(state_conv[:, sl], (0, 3, 2, 1)))
        m["shT"] = f(np.transpose(state_h[:, sl], (0, 2, 1)))
        m["pt"] = np.ascontiguousarray(page_table[sl].reshape(1, NS * NPG))
        in_maps.append(m)
    if "nc" not in _NC_CACHE:
        _NC_CACHE["nc"] = build_program()
    res = run_bass_kernel_spmd(_NC_CACHE["nc"], in_maps, core_ids=list(range(NCORES)))
    return assemble(res.results)


def assemble(R):
    B = len(R)
    cat = lambda xs: np.concatenate(xs, axis=0)
    y_prompt = np.stack([R[c]["yT"][:, :T].T for c in range(B)])
    y_sample = cat([R[c]["yT"][:, T:].T for c in range(B)])[:, None, :]
    kp = np.stack([np.stack([R[c]["kT"][l][:, :T].T for c in range(B)]) for l in range(DEPTH)]).reshape(DEPTH, B, T, NH, DH)
    vp = np.stack([np.stack([R[c]["v"][l][:T] for c in range(B)]) for l in range(DEPTH)]).reshape(DEPTH, B, T, NH, DH)
    lp = np.stack([np.stack([R[c]["lfT"][l][:, :T].T for c in range(B)]) for l in range(DEPTH)])
    cp = np.stack([np.stack([R[c]["cvT"][l].T for c in range(B)]) for l in range(DEPTH)])
    hp = np.stack([np.stack([R[c]["hT"][l][:, 0] for c in range(B)]) for l in range(DEPTH)])
    ks = np.stack([cat([R[c]["kT"][l][:, T:].T for c in range(B)]) for l in range(DEPTH)]).reshape(DEPTH, B * NS, 1, NH, DH)
    vs = np.stack([cat([R[c]["v"][l][T:] for c in range(B)]) for l in range(DEPTH)]).reshape(DEPTH, B * NS, 1, NH, DH)
    ls = np.stack([cat([R[c]["lfT"][l][:, T:].T for c in range(B)]) for l in range(DEPTH)]).reshape(DEPTH, B * NS, 1, NH)
    cs = np.stack([cat([np.transpose(R[c]["cvS"][l], (2, 1, 0)) for c in range(B)]) for l in range(DEPTH)])
    hs = np.stack([cat([R[c]["hT"][l][:, 1:].T for c in range(B)]) for l in range(DEPTH)])
    c32 = lambda a: np.ascontiguousarray(a, dtype=np.float32)
    return tuple(c32(a) for a in (y_prompt, y_sample, kp, vp, lp, cp, hp, ks, vs, ls, cs, hs))
```
